# Optimizing a Trainium2 kernel written in Bass

```python
import math
import jax
import jax.numpy as jnp
from jax import lax
import numpy as np

D_MODEL = 2048
BATCH = 8
SEQ = 4096
DEPTH = 4

CHUNK = 64
N_MEM = 256
EPS = 1e-6

HG_HEADS = 4
HG_DK = 128
HG_DV = 128
HG_QK = HG_HEADS * HG_DK
HG_W = HG_HEADS * HG_DV

RET_HEADS = 4
RET_DK = 128
RET_DV = 128
RET_QK = RET_HEADS * RET_DK
RET_W = RET_HEADS * RET_DV
ROPE_BASE = 10000.0

SSM_HEADS = 16
SSM_HEADDIM = 64
SSM_W = SSM_HEADS * SSM_HEADDIM
SSM_STATE = 128
SSM_GROUPS = 4
SSM_HPG = SSM_HEADS // SSM_GROUPS
SSM_CONV = 4
SSM_CONV_CH = SSM_W + 2 * SSM_GROUPS * SSM_STATE

MIX_W = HG_W + RET_W + SSM_W
MIX_SPLITS = (HG_QK, HG_QK, HG_W, HG_W, RET_QK, RET_QK, RET_W, RET_W, SSM_W, SSM_CONV_CH, SSM_HEADS)
IN_COLS = 2 * HG_QK + 2 * HG_W + 2 * RET_QK + 2 * RET_W + SSM_W + SSM_CONV_CH + SSM_HEADS

XA_HEADS = 4
XA_HEADDIM = 128
XA_W = XA_HEADS * XA_HEADDIM

FFN_DIM = 5632
FFN_CONV = 3

kernel_name = 'hybrid_hgrn2_retnet_mamba2_stream_encoder'


def rmsnorm(x, w):
    xf = x.astype(jnp.float32)
    y = xf * lax.rsqrt(jnp.mean(xf * xf, axis=-1, keepdims=True) + EPS)
    return (y * w.astype(jnp.float32)).astype(x.dtype)


def grouped_rmsnorm(x, w, n_groups):
    shp = x.shape
    xf = x.astype(jnp.float32).reshape(shp[:-1] + (n_groups, shp[-1] // n_groups))
    y = xf * lax.rsqrt(jnp.mean(xf * xf, axis=-1, keepdims=True) + EPS)
    return (y.reshape(shp) * w.astype(jnp.float32)).astype(x.dtype)


def causal_dwconv(x, w, b):
    K, C = w.shape
    y = lax.conv_general_dilated(x, w[:, None, :].astype(x.dtype), window_strides=(1,),
                                 padding=((K - 1, 0),), dimension_numbers=('NWC', 'WIO', 'NWC'),
                                 feature_group_count=C)
    return y + b.astype(x.dtype)


def rotary(x):
    T, Dh = x.shape[1], x.shape[-1]
    half = Dh // 2
    inv = ROPE_BASE ** (-jnp.arange(half, dtype=jnp.float32) / half)
    ang = jnp.arange(T, dtype=jnp.float32)[:, None] * inv[None, :]
    cos = jnp.cos(ang)[None, :, None, :].astype(x.dtype)
    sin = jnp.sin(ang)[None, :, None, :].astype(x.dtype)
    x1, x2 = x[..., :half], x[..., half:]
    return jnp.concatenate([x1 * cos - x2 * sin, x1 * sin + x2 * cos], axis=-1)


def chunked_gated_recurrence(q, k, v, log_f):
    Bn, T, H, K = q.shape
    V = v.shape[-1]
    N = T // CHUNK

    def to_chunks(a):
        return a.reshape(Bn, N, CHUNK, H, a.shape[-1]).transpose(1, 0, 3, 2, 4)

    causal = jnp.tril(jnp.ones((CHUNK, CHUNK), dtype=bool))

    def step(S, inp):
        qn, kn, vn, gn = inp
        b = jnp.cumsum(gn, axis=2)
        seg = jnp.where(causal[:, :, None], b[:, :, :, None, :] - b[:, :, None, :, :], -jnp.inf)
        A = jnp.einsum('bhck,bhsk,bhcsk->bhcs', qn, kn, jnp.exp(seg).astype(qn.dtype))
        o = (jnp.einsum('bhcs,bhsv->bhcv', A, vn)
             + jnp.einsum('bhck,bhkv->bhcv', qn * jnp.exp(b).astype(qn.dtype), S))
        b_last = b[:, :, -1:]
        S = (jnp.exp(b_last[:, :, 0])[..., None].astype(S.dtype) * S
             + jnp.einsum('bhsk,bhsv->bhkv', kn * jnp.exp(b_last - b).astype(kn.dtype), vn))
        return S, o

    S0 = jnp.zeros((Bn, H, K, V), q.dtype)
    _, o = lax.scan(step, S0, (to_chunks(q), to_chunks(k), to_chunks(v), to_chunks(log_f)))
    return o.transpose(1, 0, 3, 2, 4).reshape(Bn, T, H, V)


def chunked_decay_recurrence(q, k, v, log_a):
    Bn, T, G, K = q.shape
    R, V = v.shape[3], v.shape[4]
    N = T // CHUNK
    qc = q.reshape(Bn, N, CHUNK, G, K)
    kc = k.reshape(Bn, N, CHUNK, G, K)
    vc = v.reshape(Bn, N, CHUNK, G, R, V)
    cum = jnp.cumsum(log_a.astype(jnp.float32).reshape(Bn, N, CHUNK, G, R), axis=2)
    cum_h = cum.transpose(0, 1, 3, 4, 2)
    causal = jnp.tril(jnp.ones((CHUNK, CHUNK), dtype=bool))
    seg = jnp.where(causal, cum_h[..., :, None] - cum_h[..., None, :], -jnp.inf)
    decay = jnp.exp(seg).astype(q.dtype)
    scores = jnp.einsum('bncgk,bnsgk->bngcs', qc, kc)
    o_intra = jnp.einsum('bngcs,bngrcs,bnsgrv->bncgrv', scores, decay, vc)
    w_end = jnp.exp(cum[:, :, -1:] - cum).astype(q.dtype)
    U = jnp.einsum('bnsgk,bnsgr,bnsgrv->bngrkv', kc, w_end, vc)
    chunk_decay = jnp.exp(cum[:, :, -1]).astype(q.dtype)

    def step(S, inp):
        U_n, d_n = inp
        return d_n[..., None, None] * S + U_n, S

    S0 = jnp.zeros((Bn, G, R, K, V), q.dtype)
    _, S_in = lax.scan(step, S0, (jnp.moveaxis(U, 1, 0), jnp.moveaxis(chunk_decay, 1, 0)))
    S_in = jnp.moveaxis(S_in, 0, 1)
    o_inter = jnp.einsum('bncgk,bncgr,bngrkv->bncgrv', qc, jnp.exp(cum).astype(q.dtype), S_in)
    return (o_intra + o_inter).reshape(Bn, T, G, R, V)


def hybrid_mixer(h, lb, w_in, w_out, hg_norm, ret_norm, conv_w, conv_b, dt_bias, A_log, D_skip, ssm_norm):
    Bn, T, _ = h.shape
    proj = h @ w_in
    split_idx = [int(s) for s in np.cumsum(MIX_SPLITS)[:-1]]
    hq, hf, hi, hg, rq, rk, rv, rg, z, xbc, dt = jnp.split(proj, split_idx, axis=-1)

    hf32 = hf.astype(jnp.float32)
    log_f = jnp.log(lb + (1.0 - lb) * jax.nn.sigmoid(hf32))
    k_hg = ((1.0 - lb) * jax.nn.sigmoid(-hf32)).astype(h.dtype)
    q_hg = jax.nn.silu(hq)
    o_hg = chunked_gated_recurrence(q_hg.reshape(Bn, T, HG_HEADS, HG_DK), k_hg.reshape(Bn, T, HG_HEADS, HG_DK),
                                    hi.reshape(Bn, T, HG_HEADS, HG_DV), log_f.reshape(Bn, T, HG_HEADS, HG_DK))
    hg_out = grouped_rmsnorm(o_hg.reshape(Bn, T, HG_W), hg_norm, HG_HEADS) * jax.nn.sigmoid(hg)

    log_gamma = jnp.log(1.0 - 2.0 ** (-5.0 - jnp.arange(RET_HEADS, dtype=jnp.float32)))
    q_r = rotary(rq.reshape(Bn, T, RET_HEADS, RET_DK))
    k_r = rotary(rk.reshape(Bn, T, RET_HEADS, RET_DK)) * (RET_DK ** -0.5)
    log_a_r = jnp.broadcast_to(log_gamma[None, None, :, None], (Bn, T, RET_HEADS, 1))
    o_r = chunked_decay_recurrence(q_r, k_r, rv.reshape(Bn, T, RET_HEADS, 1, RET_DV), log_a_r)
    ret_out = grouped_rmsnorm(o_r.reshape(Bn, T, RET_W), ret_norm, RET_HEADS) * jax.nn.silu(rg)

    xbc = jax.nn.silu(causal_dwconv(xbc, conv_w, conv_b))
    xs = xbc[..., :SSM_W]
    Bm = xbc[..., SSM_W:SSM_W + SSM_GROUPS * SSM_STATE].reshape(Bn, T, SSM_GROUPS, SSM_STATE)
    Cm = xbc[..., SSM_W + SSM_GROUPS * SSM_STATE:].reshape(Bn, T, SSM_GROUPS, SSM_STATE)
    dt_s = jax.nn.softplus(dt.astype(jnp.float32) + dt_bias.astype(jnp.float32))
    A = -jnp.exp(A_log.astype(jnp.float32))
    log_a_s = (dt_s * A).reshape(Bn, T, SSM_GROUPS, SSM_HPG)
    xh = xs.reshape(Bn, T, SSM_GROUPS, SSM_HPG, SSM_HEADDIM)
    v_s = xh * dt_s.reshape(Bn, T, SSM_GROUPS, SSM_HPG, 1).astype(xh.dtype)
    y = chunked_decay_recurrence(Cm, Bm, v_s, log_a_s)
    y = y + D_skip.reshape(SSM_GROUPS, SSM_HPG, 1).astype(xh.dtype) * xh
    ssm_out = grouped_rmsnorm(y.reshape(Bn, T, SSM_W) * jax.nn.silu(z), ssm_norm, SSM_GROUPS)

    return jnp.concatenate([hg_out, ret_out, ssm_out], axis=-1) @ w_out


def memory_cross_attention(h, mem_n, wq, wkv, wo):
    Bn, T, _ = h.shape
    q = (h @ wq).reshape(Bn, T, XA_HEADS, XA_HEADDIM)
    kv = mem_n @ wkv
    k = kv[..., :XA_W].reshape(Bn, N_MEM, XA_HEADS, XA_HEADDIM)
    v = kv[..., XA_W:].reshape(Bn, N_MEM, XA_HEADS, XA_HEADDIM)
    s = jnp.einsum('bthd,bmhd->bhtm', q, k).astype(jnp.float32) * (XA_HEADDIM ** -0.5)
    p = jax.nn.softmax(s, axis=-1).astype(v.dtype)
    o = jnp.einsum('bhtm,bmhd->bthd', p, v).reshape(Bn, T, XA_W)
    return o @ wo


def conv_glu_ffn(h, w_up, conv_w, conv_b, w_down):
    u = causal_dwconv(h @ w_up, conv_w, conv_b)
    gate, up = u[..., :FFN_DIM], u[..., FFN_DIM:]
    return (jax.nn.silu(gate) * up) @ w_down


def setup_inputs(seed: int = 0) -> dict:
    key = jax.random.key(seed)
    ks = jax.random.split(key, 26)
    f32 = jnp.float32

    def nrm(k, shape, scale):
        return jax.random.normal(k, shape, f32) * scale

    def gain(k, shape):
        return 1.0 + 0.02 * jax.random.normal(k, shape, f32)

    dt0 = jnp.exp(jax.random.uniform(ks[9], (DEPTH, SSM_HEADS), f32, math.log(1e-3), math.log(1e-1)))
    return {
        'x': nrm(ks[0], (BATCH, SEQ, D_MODEL), 1.0),
        'mem': nrm(ks[1], (BATCH, N_MEM, D_MODEL), 1.0),
        'w_in': nrm(ks[2], (DEPTH, D_MODEL, IN_COLS), D_MODEL ** -0.5),
        'w_out': nrm(ks[3], (DEPTH, MIX_W, D_MODEL), MIX_W ** -0.5),
        'hg_lb_logits': nrm(ks[4], (DEPTH, HG_QK), 0.5),
        'hg_norm': gain(ks[5], (DEPTH, HG_W)),
        'ret_norm': gain(ks[6], (DEPTH, RET_W)),
        'ssm_conv_w': nrm(ks[7], (DEPTH, SSM_CONV, SSM_CONV_CH), SSM_CONV ** -0.5),
        'ssm_conv_b': nrm(ks[8], (DEPTH, SSM_CONV_CH), 0.01),
        'ssm_dt_bias': dt0 + jnp.log(-jnp.expm1(-dt0)),
        'ssm_A_log': jnp.log(jax.random.uniform(ks[10], (DEPTH, SSM_HEADS), f32, 1.0, 16.0)),
        'ssm_D': 1.0 + 0.1 * jax.random.normal(ks[11], (DEPTH, SSM_HEADS), f32),
        'ssm_norm': gain(ks[12], (DEPTH, SSM_W)),
        'norm_mix': gain(ks[13], (DEPTH, D_MODEL)),
        'norm_xattn': gain(ks[14], (DEPTH, D_MODEL)),
        'norm_mem': gain(ks[15], (DEPTH, D_MODEL)),
        'xa_wq': nrm(ks[16], (DEPTH, D_MODEL, XA_W), D_MODEL ** -0.5),
        'xa_wkv': nrm(ks[17], (DEPTH, D_MODEL, 2 * XA_W), D_MODEL ** -0.5),
        'xa_wo': nrm(ks[18], (DEPTH, XA_W, D_MODEL), XA_W ** -0.5),
        'norm_ffn': gain(ks[19], (DEPTH, D_MODEL)),
        'ffn_w_up': nrm(ks[20], (DEPTH, D_MODEL, 2 * FFN_DIM), D_MODEL ** -0.5),
        'ffn_conv_w': nrm(ks[21], (DEPTH, FFN_CONV, 2 * FFN_DIM), FFN_CONV ** -0.5),
        'ffn_conv_b': nrm(ks[22], (DEPTH, 2 * FFN_DIM), 0.01),
        'ffn_w_down': nrm(ks[23], (DEPTH, FFN_DIM, D_MODEL), FFN_DIM ** -0.5),
        'norm_final': gain(ks[24], (D_MODEL,)),
    }


def reference(x, mem, w_in, w_out, hg_lb_logits, hg_norm, ret_norm, ssm_conv_w, ssm_conv_b, ssm_dt_bias,
              ssm_A_log, ssm_D, ssm_norm, norm_mix, norm_xattn, norm_mem, xa_wq, xa_wkv, xa_wo, norm_ffn,
              ffn_w_up, ffn_conv_w, ffn_conv_b, ffn_w_down, norm_final):
    p = jax.nn.softmax(hg_lb_logits.astype(jnp.float32), axis=0)
    lower_bounds = jnp.cumsum(p, axis=0) - p[0]
    for l in range(DEPTH):
        x = x + hybrid_mixer(rmsnorm(x, norm_mix[l]), lower_bounds[l], w_in[l], w_out[l], hg_norm[l],
                             ret_norm[l], ssm_conv_w[l], ssm_conv_b[l], ssm_dt_bias[l], ssm_A_log[l],
                             ssm_D[l], ssm_norm[l])
        x = x + memory_cross_attention(rmsnorm(x, norm_xattn[l]), rmsnorm(mem, norm_mem[l]),
                                       xa_wq[l], xa_wkv[l], xa_wo[l])
        x = x + conv_glu_ffn(rmsnorm(x, norm_ffn[l]), ffn_w_up[l], ffn_conv_w[l], ffn_conv_b[l], ffn_w_down[l])
    return rmsnorm(x, norm_final)
```

```python
import math
import os
import contextlib
import numpy as np
import concourse.bass as bass
import concourse.mybir as mybir
from concourse.bass_utils import run_bass_kernel_spmd

F32 = mybir.dt.float32
BF16 = mybir.dt.bfloat16
AF = mybir.ActivationFunctionType
ALU = mybir.AluOpType

D = 2048
NCH = 16
TT = 512
L_FULL = 4
T_FULL = 4096
NMEM = 256
IN_COLS = 7184
FFN = 5632
EPS = 1e-6
FFN_PARTS = [(0, 11), (11, 11), (22, 11), (33, 11)]
NSLOT = 4
WE = (D * IN_COLS + D * D + D * 512 + D * 1024 + 512 * D + D * 2 * FFN + FFN * D) // 128

SAME_ENGINE_SYNC = os.environ.get("SES", "1") == "1"
EPOCH = 30000
N_DMA_SEMS = 40


class Prog:
    ENGS = ("pe", "act", "dve", "pool", "sp")

    def __init__(self, nc, stack):
        self.nc = nc
        self.stack = stack
        self.ops = {e: [] for e in self.ENGS}
        self.nsem = 0
        self.eng_sem = {e: None for e in self.ENGS}
        self.eng_cnt = {e: 0 for e in self.ENGS}
        self.known = {e: {} for e in self.ENGS}
        self.segs = {}
        self.dma_sems = [self._new_sem("dq%d" % i) for i in range(N_DMA_SEMS)]
        self.dma_cnt = [0] * N_DMA_SEMS
        self.dma_rr = 0
        self.dma_rr_pool = N_DMA_SEMS - 12
        self.semobj = {}
        self.n_ops = 0
        self.mute = False

    def _new_sem(self, name):
        s = self.stack.enter_context(self.nc.semaphore(name))
        self.nsem += 1
        return s

    colspace = {}

    @classmethod
    def region(cls, ap):
        steps = ap.ap
        off = ap.offset
        space = str(ap.space)
        esz = 2 if ap.dtype == BF16 else 4
        E = cls.colspace.get(ap.tensor.name)
        if E is not None:
            lay, rem = divmod(off, 128 * E)
            col = rem % E
            return ("%s:%d" % (ap.tensor.name, lay), col * esz, (col + steps[-1][1]) * esz)
        if space in ("SB", "PSUM"):
            pstride = steps[0][0]
            lo = off % pstride if pstride > 0 else off
            ext = 1
            for st, cnt in steps[1:]:
                ext += (cnt - 1) * abs(st)
            return (ap.tensor.name, lo * esz, (lo + ext) * esz)
        ext = 1
        for st, cnt in steps:
            ext += (cnt - 1) * abs(st)
        return (ap.tensor.name, off * esz, (off + ext) * esz)

    @classmethod
    def psum_bank_region(cls, ap):
        name, lo, hi = cls.region(ap)
        b0 = lo // 2048
        b1 = (hi - 1) // 2048
        return (name, b0 * 2048, (b1 + 1) * 2048)

    def _collect(self, reg, is_write, deps):
        name, lo, hi = reg
        for s in self.segs.get(name, ()):
            if s[0] < hi and lo < s[1]:
                w = s[2]
                if w is not None:
                    self._add(deps, w)
                if is_write:
                    for r in s[3].values():
                        self._add(deps, r)

    @staticmethod
    def _add(deps, tok):
        k = id(tok[0])
        o = deps.get(k)
        if o is None or o[1] < tok[1]:
            deps[k] = tok

    def _commit(self, reg, is_write, tok):
        name, lo, hi = reg
        segs = self.segs.setdefault(name, [])
        out = []
        covered = []
        for s in segs:
            if s[1] <= lo or s[0] >= hi:
                out.append(s)
                continue
            if s[0] < lo:
                out.append([s[0], lo, s[2], dict(s[3])])
            if s[1] > hi:
                out.append([hi, s[1], s[2], dict(s[3])])
            a, b = max(s[0], lo), min(s[1], hi)
            covered.append((a, b))
            if is_write:
                pass
            else:
                r = dict(s[3])
                k = id(tok[0])
                o = r.get(k)
                if o is None or o[1] < tok[1]:
                    r[k] = tok
                out.append([a, b, s[2], r])
        if is_write:
            out.append([lo, hi, tok, {}])
        else:
            covered.sort()
            cur = lo
            for a, b in covered:
                if a > cur:
                    out.append([cur, a, None, {id(tok[0]): tok}])
                cur = max(cur, b)
            if cur < hi:
                out.append([cur, hi, None, {id(tok[0]): tok}])
        self.segs[name] = out

    def _next_token(self, eng):
        if self.eng_sem[eng] is None or self.eng_cnt[eng] >= EPOCH:
            self.eng_sem[eng] = self._new_sem("e_%s_%d" % (eng, self.nsem))
            self.eng_cnt[eng] = 0
        self.eng_cnt[eng] += 1
        return (self.eng_sem[eng], self.eng_cnt[eng], eng)

    def op(self, eng, fn, reads=(), writes=(), dma=False):
        if self.mute:
            return
        self.n_ops += 1
        deps = {}
        rregs = []
        wregs = []
        for a in reads:
            if str(a.space) == "PSUM":
                wregs.append(self.psum_bank_region(a))
            else:
                rregs.append(self.region(a))
        for a in writes:
            if str(a.space) == "PSUM":
                wregs.append(self.psum_bank_region(a))
            else:
                wregs.append(self.region(a))
        for r in rregs:
            self._collect(r, False, deps)
        for w in wregs:
            self._collect(w, True, deps)
        if dma:
            if eng == "pool":
                i = self.dma_rr_pool
                self.dma_rr_pool = N_DMA_SEMS - 12 + (self.dma_rr_pool + 1 - (N_DMA_SEMS - 12)) % 12
            else:
                i = self.dma_rr
                self.dma_rr = (self.dma_rr + 1) % (N_DMA_SEMS - 12)
            sem = self.dma_sems[i]
            if self.dma_cnt[i] > 0:
                self._add(deps, (sem, self.dma_cnt[i], "dma"))
            self.dma_cnt[i] += 16
            tok = (sem, self.dma_cnt[i], "dma")
            inc = 16
        else:
            tok = self._next_token(eng)
            inc = 1
        waits = []
        kn = self.known[eng]
        for t in deps.values():
            if t[2] == eng:
                if eng == "pe" or not SAME_ENGINE_SYNC:
                    continue
            k = id(t[0])
            if kn.get(k, 0) >= t[1]:
                continue
            kn[k] = t[1]
            waits.append((t[0], t[1]))
        for r in rregs:
            self._commit(r, False, tok)
        for w in wregs:
            self._commit(w, True, tok)
        self.ops[eng].append((waits, fn, tok[0], inc))

    def replay(self, eng, handle):
        for waits, fn, sem, inc in self.ops[eng]:
            for s, v in waits:
                handle.wait_ge(s, v)
            ins = fn(handle)
            ins.then_inc(sem, inc)

    def final_wait_all(self, eng_handle_name="sp"):
        waits = []
        for e in self.ENGS:
            if self.eng_sem[e] is not None:
                waits.append((self.eng_sem[e], self.eng_cnt[e]))
        for i, s in enumerate(self.dma_sems):
            if self.dma_cnt[i] > 0:
                waits.append((s, self.dma_cnt[i]))
        return waits

    def mm(self, out, lhsT, rhs, start=True, stop=True):
        self.op("pe", lambda e: e.matmul(out, lhsT, rhs, start=start, stop=stop),
                reads=[lhsT, rhs], writes=[out])

    def tr(self, out, in_, ident):
        self.op("pe", lambda e: e.transpose(out, in_, ident), reads=[in_, ident], writes=[out])

    def act(self, out, in_, func, bias=None, scale=None, accum_out=None, eng="act"):
        reads = [in_]
        kw = {}
        if bias is not None:
            kw["bias"] = bias
            if not isinstance(bias, (int, float)):
                reads.append(bias)
        if scale is not None:
            kw["scale"] = scale
            if not isinstance(scale, (int, float)):
                reads.append(scale)
        writes = [out]
        if accum_out is not None:
            kw["accum_out"] = accum_out
            writes.append(accum_out)
        self.op("act", lambda e: e.activation(out, in_, func, **kw), reads=reads, writes=writes)

    def tt(self, out, in0, in1, op, eng="dve"):
        self.op(eng, lambda e: e.tensor_tensor(out, in0, in1, op), reads=[in0, in1], writes=[out])

    def ts(self, out, in0, s1, op0, s2=None, op1=None, eng="dve"):
        reads = [in0]
        if not isinstance(s1, (int, float)):
            reads.append(s1)
        if s2 is not None and not isinstance(s2, (int, float)):
            reads.append(s2)
        if op1 is None:
            self.op(eng, lambda e: e.tensor_scalar(out, in0, s1, None, op0), reads=reads, writes=[out])
        else:
            self.op(eng, lambda e: e.tensor_scalar(out, in0, s1, s2, op0, op1), reads=reads, writes=[out])

    def stt(self, out, in0, scalar, in1, op0, op1):
        reads = [in0, in1]
        if not isinstance(scalar, (int, float)):
            reads.append(scalar)
        self.op("dve", lambda e: e.scalar_tensor_tensor(out, in0, scalar, in1, op0, op1),
                reads=reads, writes=[out])

    def copy(self, out, in_, eng="act"):
        if eng == "act":
            self.op("act", lambda e: e.copy(out, in_), reads=[in_], writes=[out])
        else:
            self.op(eng, lambda e: e.tensor_copy(out, in_), reads=[in_], writes=[out])

    def memset(self, ap, val, eng="pool"):
        self.op(eng, lambda e: e.memset(ap, val), reads=[], writes=[ap])

    def recip(self, out, in_):
        self.op("dve", lambda e: e.reciprocal(out, in_), reads=[in_], writes=[out])

    def scan(self, out, d0, d1, initial, op0, op1):
        self.op("dve", lambda e: e.tensor_tensor_scan(out, d0, d1, initial, op0, op1),
                reads=[d0, d1], writes=[out])

    def dma(self, out, in_, eng="sp"):
        self.op(eng, lambda e: e.dma_start(out=out, in_=in_), reads=[in_], writes=[out], dma=True)


def host_constants(T):
    c = {}
    i = np.arange(128)
    c["ident_bf"] = np.eye(128, dtype=np.float32)
    c["ident_f"] = np.eye(128, dtype=np.float32)
    c["ones_f"] = np.ones((128, 128), np.float32)
    c["tri_f"] = (i[:, None] <= i[None, :]).astype(np.float32)
    c["strict_f"] = (i[:, None] > i[None, :]).astype(np.float32)
    neg = np.where(i[None, :] < i[:, None], -30000.0, 0.0).astype(np.float32)
    c["neg4"] = np.tile(neg, (1, 4))
    j = np.arange(64)
    c["causal64"] = (j[None, :] >= j[:, None]).astype(np.float32)
    gam = 1.0 - 2.0 ** (-5.0 - np.arange(4, dtype=np.float64))
    lg = np.log(gam.astype(np.float32)).astype(np.float32).astype(np.float64)
    dm = np.zeros((128, 4, 128), np.float64)
    for h in range(4):
        dm[:, h, :] = np.where(i[None, :] >= i[:, None], np.exp(lg[h] * (i[None, :] - i[:, None])), 0.0)
    c["ret_dmask"] = (dm * 128 ** -0.5).astype(np.float32)
    c["ret_grow"] = np.broadcast_to(np.exp(lg[None, :, None] * (i[None, None, :] + 1.0)), (128, 4, 128)).astype(np.float32)
    c["ret_kscale"] = (np.exp(lg[None, :] * (127.0 - i[:, None])) * 128 ** -0.5).astype(np.float32)
    c["ret_g128"] = [float(np.exp(lg[h] * 128.0)) for h in range(4)]
    prot = np.zeros((128, 128), np.float32)
    for m in range(128):
        prot[(m + 64) % 128, m] = 1.0
    c["protT"] = prot
    half = 64
    inv = (10000.0 ** (-np.arange(half, dtype=np.float32) / half)).astype(np.float32)
    ang = np.arange(T, dtype=np.float32)[:, None] * inv[None, :]
    cos = np.cos(ang).astype(np.float32).T
    sin = np.sin(ang).astype(np.float32).T
    c["rot_cos"] = np.concatenate([cos, cos], 0)
    c["rot_sin"] = np.concatenate([-sin, sin], 0)
    rm = np.ones((128, TT), np.float32)
    rm[:, ::64] = 0.0
    c["resetmask"] = rm
    return c


def build(L=L_FULL, T=T_FULL, taps=None, phases=("mixer", "xattn", "ffn")):
    NT = T // TT
    nc = bass.Bass("TRN2", target_bir_lowering=False)
    hc = host_constants(T)
    taps = taps or ()
    subs = ("hgrn", "ret", "ssd") if "mixer" in phases else tuple(p for p in phases if p in ("hgrn", "ret", "ssd"))

    def din(name, shape, dt=F32):
        return nc.dram_tensor(name, list(shape), dt, kind="ExternalInput").ap()

    x_in = din("x_t", [NT, 128, NCH, TT])
    mem_in = din("mem_t", [128, NCH, NMEM])
    wsf = din("wstream", [L, 128, WE])
    pv = din("pvec", [128, L, 4, NCH])
    pfin = din("pfin", [128, NCH])
    plb = din("plb", [128, L_FULL, 4])
    phn = din("phn", [128, L, 2, 4])
    pcw = din("pcw", [128, L, NCH, 5])
    prow = din("prow", [L, 3, 16])
    psn = din("psn", [L, 1024])
    pfw = din("pfw", [L, 128, 88, 4])
    cin = {}
    for k in ("ident_f", "ones_f", "tri_f", "strict_f", "neg4", "causal64", "ret_dmask", "ret_grow",
              "ret_kscale", "protT", "rot_cos", "rot_sin", "resetmask"):
        cin[k] = din("k_" + k, hc[k].shape)
    y_out = nc.dram_tensor("y_t", [NT, 128, NCH, TT], F32, kind="ExternalOutput").ap()
    tap_out = {}

    def dint(name, shape, dt):
        return nc.dram_tensor(name, list(shape), dt, kind="Internal").ap()

    xs_d = dint("xs_d", [NT, 128, NCH, TT], F32)
    wsb = [dint("wstream_b%d" % l, [128, WE], BF16) for l in range(L)]
    Prog.colspace = {"wstream_b%d" % l: WE for l in range(L)}

    stack = contextlib.ExitStack()
    with stack:
        P = Prog(nc, stack)

        def sb(name, shape, dt=F32):
            return stack.enter_context(nc.sbuf_tensor(name, list(shape), dt))

        X = sb("X", [128, NCH, TT])
        XN = sb("XN", [128, NCH, TT], BF16)
        MIX = sb("MIX", [128, 16, TT], BF16)
        BIG = sb("BIG", [128, 34 * TT], BF16)
        WS = sb("WS", [128, NSLOT, 4096], BF16)
        FT = sb("FT", [128, 6, TT + 4])
        ST = sb("ST", [128, 1024])
        ST2 = sb("ST2", [128, 2048], BF16)
        SBF = sb("SBF", [128, 2048], BF16)
        SMB = sb("SMB", [128, 4, 128], BF16)
        HR = sb("HR", [128, 4, 128])
        RS = sb("RS", [128, 4, 128])
        SS = sb("SS", [128, 1024])
        KT = sb("KT", [128, 4, NMEM], BF16)
        VV = sb("VV", [128, 2, 512], BF16)
        CF = sb("CF", [128, 88, 2])
        CS = sb("CS", [128, NCH, 3])
        SM = sb("SM", [128, 256])
        c_ident_bf = sb("c_ident_bf", [128, 128], BF16)
        c_ones_bf = sb("c_ones_bf", [128, 128], BF16)
        c_neg4_bf = sb("c_neg4_bf", [128, 512], BF16)
        c_prot_bf = sb("c_prot_bf", [128, 128], BF16)
        c_reset = sb("c_reset", [128, TT], BF16)
        c_ident_f = sb("c_ident_f", [128, 128])
        c_ones_f = sb("c_ones_f", [128, 128])
        c_tri_f = sb("c_tri_f", [128, 128])
        c_strict_f = sb("c_strict_f", [128, 128])
        c_causal64 = sb("c_causal64", [64, 64])
        c_dmask = sb("c_dmask", [128, 4, 128])
        c_grow = sb("c_grow", [128, 4, 128])
        c_kscale = sb("c_kscale", [128, 4])
        PV = sb("PV", [128, L, 4, NCH])
        PFIN = sb("PFIN", [128, NCH])
        PLB = sb("PLB", [128, L_FULL, 4])
        LB = sb("LB", [128, L_FULL, 2, 4])
        PHN = sb("PHN", [128, L, 2, 4])
        PCW = sb("PCW", [128, L, NCH, 5])
        PROW = sb("PROW", [128, L, 3, 16])
        PSN = sb("PSN", [128, 1024])
        PFW = sb("PFW", [128, 88, 4])
        DPREV = sb("DPREV", [128, 4])

        PF = stack.enter_context(nc.psum_tensor("PF", [128, 7, 512], F32))
        PB = stack.enter_context(nc.psum_tensor("PB", [128, 1024], BF16))

        IB0, IT0, BC0 = 0, 10 * TT, 26 * TT

        def ibc(i, n=1):
            return BIG[:, IB0 + i * TT:IB0 + (i + n) * TT]

        def IT(a, b, parts=128):
            return BIG[0:parts, IT0 + a:IT0 + b]

        BCv = BIG[:, BC0:BC0 + 8 * TT].rearrange("p (c t) -> p c t", t=TT)
        HID = BIG[:, 0:11 * TT].rearrange("p (c t) -> p c t", t=TT)

        MUL, ADD, SUB = ALU.mult, ALU.add, ALU.subtract

        KCUT = int(os.environ.get("KCUT", "0"))
        marks = []
        build.marks = marks

        def mark(name):
            marks.append((name, len([o for o in P.ops["pe"]])))

        def cut(n):
            if KCUT == n:
                P.mute = True

        def tap(name, ap):
            if name not in taps:
                return
            shp = list(ap.shape)
            t = nc.dram_tensor("tap_" + name, shp, ap.dtype, kind="ExternalOutput").ap()
            tap_out[name] = t
            P.dma(t, ap)

        stage = FT[:, 0, 0:512]
        stage2 = FT[:, 1, 0:512]
        P.dma(c_ident_f[:], cin["ident_f"])
        P.dma(c_ones_f[:], cin["ones_f"])
        P.dma(c_tri_f[:], cin["tri_f"])
        P.dma(c_strict_f[:], cin["strict_f"])
        P.dma(c_causal64[:], cin["causal64"])
        P.dma(c_dmask[:], cin["ret_dmask"])
        P.dma(c_grow[:], cin["ret_grow"])
        P.dma(c_kscale[:], cin["ret_kscale"])
        P.copy(c_ident_bf[:], c_ident_f[:], eng="dve")
        P.copy(c_ones_bf[:], c_ones_f[:], eng="dve")
        P.dma(stage, cin["neg4"])
        P.copy(c_neg4_bf[:], stage, eng="dve")
        P.dma(stage2[:, 0:128], cin["protT"])
        P.copy(c_prot_bf[:], stage2[:, 0:128], eng="dve")
        stage3 = FT[:, 2, 0:512]
        P.dma(stage3, cin["resetmask"])
        P.copy(c_reset[:], stage3, eng="dve")
        P.dma(PV[:], pv)
        P.dma(PFIN[:], pfin)
        P.dma(PLB[:], plb)
        P.dma(PHN[:], phn)
        P.dma(PCW[:], pcw)
        for l in range(L):
            P.dma(PROW[:, l, :, :].rearrange("p a b -> p (a b)"),
                  prow[l].rearrange("a b -> (a b)").partition_broadcast(128))
        for l in range(L):
            P.act(PROW[:, l, 1, :], PROW[:, l, 1, :], AF.Exp)
            P.ts(PROW[:, l, 1, :], PROW[:, l, 1, :], -1.0, MUL)
        EL = SM[:, 0:16].rearrange("p (a b) -> p a b", b=4)
        P.act(EL, PLB[:], AF.Exp)
        P.tt(SM[:, 16:20], EL[:, 0, :], EL[:, 1, :], ADD)
        P.tt(SM[:, 16:20], SM[:, 16:20], EL[:, 2, :], ADD)
        P.tt(SM[:, 16:20], SM[:, 16:20], EL[:, 3, :], ADD)
        P.recip(SM[:, 20:24], SM[:, 16:20])
        for l in range(L_FULL):
            P.tt(EL[:, l, :], EL[:, l, :], SM[:, 20:24], MUL)
        P.memset(LB[:, 0, 0, :], 0.0, eng="dve")
        for l in range(1, L_FULL):
            P.tt(LB[:, l, 0, :], LB[:, l - 1, 0, :], EL[:, l, :], ADD)
        for l in range(L_FULL):
            P.ts(LB[:, l, 1, :], LB[:, l, 0, :], -1.0, MUL, 1.0, ADD)

        CASTW = 8192
        for l in range(L):
            for a in range(0, WE, CASTW):
                b_ = min(WE, a + CASTW)
                P.dma(wsb[l][:, a:b_], wsf[l, :, a:b_], eng="pool")

        wstate = {"i": 0, "next": 0}
        wreg = {}
        build.wreg = wreg

        def wload(wname, l, c0, ncols, r0=0, KC=NCH):
            key = (wname, r0, KC, c0, ncols)
            n = KC * ncols
            assert n <= 4096
            if key not in wreg:
                wreg[key] = wstate["next"]
                wstate["next"] += n
            off = wreg[key]
            s = wstate["i"] % NSLOT
            wstate["i"] += 1
            P.dma(WS[:, s, 0:n], wsb[l][:, off:off + n])
            return WS[:, s, 0:n].rearrange("p (k n) -> p k n", n=ncols)

        def fm_slice(wname, l, c0, nchunks, rhs3, KC=NCH, r0=0, cpb=2):
            for b0 in range(0, nchunks, cpb):
                nb_ = min(cpb, nchunks - b0)
                wv = wload(wname, l, c0 + b0 * 128, nb_ * 128, r0=r0, KC=KC)
                for ci in range(nb_):
                    yield b0 + ci, fm_chunk(wv, ci, rhs3, nb(), KC=KC)

        def tm_slice(wname, l, c0, ncols_total, M, ntb, cpb=256):
            for b0 in range(0, ncols_total, cpb):
                wv = wload(wname, l, c0 + b0, cpb)
                for tb in range(ntb):
                    yield tb, b0, tm_block(wv, tb * M, M, nb(), cpb)

        def rmsnorm_fm(src, dst, wcol, ncols, nch=NCH):
            sq = ibc(0)[:, 0:ncols]
            acc = PF[:, 3, 0:ncols]
            for k in range(nch):
                P.act(sq, src[:, k, :], AF.Square)
                P.mm(acc, c_ones_bf[:], sq, start=(k == 0), stop=(k == nch - 1))
            rstd = FT[:, 5, 0:ncols]
            P.act(rstd, acc, AF.Sqrt, bias=EPS, scale=1.0 / (nch * 128))
            P.recip(rstd, rstd)
            if dst is not None:
                for k in range(nch):
                    P.stt(dst[:, k, :], src[:, k, :], wcol[:, k:k + 1], rstd, MUL, MUL)
            return rstd

        def fm_chunk(wv, ci, rhs3, bank, KC=NCH, ncols=TT):
            out = PF[:, bank, 0:ncols]
            for k in range(KC):
                P.mm(out, wv[:, k, ci * 128:(ci + 1) * 128], rhs3[:, k, :], start=(k == 0), stop=(k == KC - 1))
            return out

        def tm_block(wv, tok0, M, bank, ncols, c0=0):
            out = PF[0:M, bank, 0:ncols]
            for k in range(NCH):
                P.mm(out, XN[:, k, tok0:tok0 + M], wv[:, k, c0:c0 + ncols], start=(k == 0), stop=(k == NCH - 1))
            return out

        bank_rr = {"i": 0}

        NB_BANKS = (0, 1, 2, 4, 5, 6)

        def nb():
            b = NB_BANKS[bank_rr["i"] % len(NB_BANKS)]
            bank_rr["i"] += 1
            return b

        def headnorm_out(o_ps, gate, nw, dst, bank=3):
            sq = ibc(0)
            P.act(sq, o_ps, AF.Square)
            ss = PF[:, bank, :]
            P.mm(ss, c_ones_bf[:], sq)
            rstd = FT[:, 5, 0:TT]
            P.act(rstd, ss, AF.Sqrt, bias=EPS, scale=1.0 / 128)
            P.recip(rstd, rstd)
            tmp = FT[:, 4, 0:TT]
            P.tt(tmp, o_ps, rstd, MUL)
            P.stt(dst, tmp, nw, gate, MUL, MUL)

        def wout_partial(l, r0, KC):
            for m, ps in fm_slice("w_out", l, 0, NCH, MIX[:, r0:r0 + KC, :], KC=KC, r0=r0, cpb=(8 if KC == 4 else 4)):
                P.tt(X[:, m, :], X[:, m, :], ps, ADD)

        OH = [PF[:, 4, :], PF[:, 5, :]]
        GG = BCv[:, 0:4, :]

        for l in range(L):
            last = (l == L - 1)
            src_d = x_in if l == 0 else xs_d
            P.memset(HR[:], 0.0)
            P.memset(RS[:], 0.0)
            P.memset(SS[:], 0.0)
            P.memset(CF[:], 0.0)
            P.memset(CS[:], 0.0)
            P.memset(DPREV[:], 1.0)
            P.dma(PSN[:], psn[l].partition_broadcast(128))
            P.dma(PFW[:], pfw[l])
            if "xattn" in phases:
                memx = X[:, :, 0:NMEM]
                P.dma(memx, mem_in)
                memn = XN[:, :, 0:NMEM]
                rmsnorm_fm(memx, memn, PV[:, l, 2, :], NMEM)
                for hb in range(2):
                    wk_v = wload("xa_wkv", l, hb * 256, 256)
                    for ci in range(2):
                        ps = fm_chunk(wk_v, ci, memn, nb(), ncols=NMEM)
                        P.copy(KT[:, hb * 2 + ci, :], ps)
                for vb in range(2):
                    wv_v = wload("xa_wkv", l, 512 + vb * 256, 256)
                    for mb in range(2):
                        out = PF[:, nb(), 0:256]
                        for k in range(NCH):
                            P.mm(out, memn[:, k, mb * 128:(mb + 1) * 128], wv_v[:, k, :], start=(k == 0), stop=(k == NCH - 1))
                        P.copy(VV[:, mb, vb * 256:(vb + 1) * 256], out)

            for ti in range(NT):
                t0 = ti * TT
                for m in range(NCH):
                    P.dma(X[:, m, :], src_d[ti, :, m, :])

                if subs:
                    rmsnorm_fm(X[:], XN[:], PV[:, l, 0, :], TT)
                    if l == 0 and ti == 0:
                        tap("xn", XN[:])
                    if "hgrn" in subs:
                        mark("hgrn_proj")
                        QT = ibc(1, 4).rearrange("p (h t) -> p h t", t=TT)
                        KTl = ibc(5, 4).rearrange("p (h t) -> p h t", t=TT)
                        VT = IT(0, 4096, 64).rearrange("p (c n) -> p c n", n=512)
                        for (h, psq), (_, psf) in zip(fm_slice("w_in", l, 0, 4, XN), fm_slice("w_in", l, 512, 4, XN)):
                            qs = FT[:, 0, 0:TT]
                            P.act(qs, psq, AF.Silu)
                            f = FT[:, 1, 0:TT]
                            P.act(f, psf, AF.Sigmoid)
                            P.ts(f, f, LB[:, l, 1, h:h + 1], MUL, LB[:, l, 0, h:h + 1], ADD)
                            g = FT[:, 2, 0:TT]
                            P.act(g, f, AF.Ln)
                            kk = FT[:, 3, 0:TT]
                            P.ts(kk, f, -1.0, MUL, 1.0, ADD)
                            b = FT[:, 1, 0:TT]
                            P.scan(b, c_reset[:], g, 0.0, MUL, ADD)
                            b3 = b.rearrange("p (c t) -> p c t", t=64)
                            bm = SM[:, 32:40]
                            P.copy(bm, b3[:, :, 31], eng="dve")
                            P.tt(b3, b3, bm.unsqueeze(2).to_broadcast([128, 8, 64]), SUB)
                            EM = SM[:, 40:48]
                            Dd = SM[:, 48:56]
                            Ee = SM[:, 64 + h * 8:64 + h * 8 + 8]
                            P.act(EM, bm, AF.Exp)
                            P.act(Dd, b3[:, :, 63], AF.Exp)
                            P.tt(Ee[:, 0:1], EM[:, 0:1], DPREV[:, h:h + 1], MUL)
                            P.tt(Ee[:, 1:8], EM[:, 1:8], Dd[:, 0:7], MUL)
                            P.copy(DPREV[:, h:h + 1], Dd[:, 7:8], eng="dve")
                            e1 = FT[:, 2, 0:TT]
                            P.act(e1, b, AF.Exp)
                            P.tt(QT[:, h, :], qs, e1, MUL)
                            e2 = FT[:, 4, 0:TT]
                            P.act(e2, b, AF.Exp, scale=-1.0)
                            P.tt(KTl[:, h, :], kk, e2, MUL)
                        for c, b0, ps in tm_slice("w_in", l, 1024, 512, 64, 8):
                            P.copy(VT[:, c, b0:b0 + 256], ps)
                        for h, ps in fm_slice("w_in", l, 1536, 4, XN):
                            P.act(GG[:, h, :], ps, AF.Sigmoid)
                        mark("hgrn_batch")
                        KTM = IT(4096, 8192, 64).rearrange("p (h c n) -> p h c n", h=4, c=8)
                        AB = SBF[0:64, 0:2048].rearrange("p (h c n) -> p h c n", h=4, c=8)
                        for h in range(4):
                            pb = PB[0:64, :].rearrange("p (c n) -> p c n", n=128)
                            for c in range(8):
                                P.tr(pb[:, c, :], KTl[:, h, c * 64:(c + 1) * 64], c_ident_bf[:])
                            P.copy(KTM[:, h, :, :], pb)
                            pa = PF[0:64, 6, :].rearrange("p (c n) -> p c n", n=64)
                            for c in range(8):
                                P.mm(pa[:, c, :], KTl[:, h, c * 64:(c + 1) * 64], QT[:, h, c * 64:(c + 1) * 64])
                            P.tt(AB[:, h, :, :], pa, c_causal64[:].unsqueeze(1).to_broadcast([64, 8, 64]), MUL)
                        mark("hgrn_rec")
                        SM8 = ST2[:, :].rearrange("p (b c n) -> p b c n", b=2, c=8)

                        def hg_pp(h):
                            buf = h % 2
                            for c in range(8):
                                P.mm(PF[:, 2 * buf + c // 4, (c % 4) * 128:(c % 4 + 1) * 128],
                                     KTM[:, h, c, :], VT[:, c, h * 128:(h + 1) * 128])
                            for hf in range(2):
                                dst = ST[:, hf * 512:(hf + 1) * 512] if buf == 0 else FT[:, hf, 0:512]
                                P.copy(dst, PF[:, 2 * buf + hf, :])

                        def hg_chain(h):
                            buf = h % 2
                            Ee = SM[:, 64 + h * 8:64 + h * 8 + 8]
                            for c in range(8):
                                pps = ST[:, c * 128:(c + 1) * 128] if buf == 0 else FT[:, c // 4, (c % 4) * 128:(c % 4 + 1) * 128]
                                P.ts(SM8[:, buf, c, :], HR[:, h, :], Ee[:, c:c + 1], MUL)
                                P.stt(HR[:, h, :], HR[:, h, :], Ee[:, c:c + 1], pps, MUL, ADD)

                        def hg_o(h):
                            buf = h % 2
                            for c in range(8):
                                oc = OH[buf][:, c * 64:(c + 1) * 64]
                                P.mm(oc, VT[:, c, h * 128:(h + 1) * 128], AB[:, h, c, :], start=True, stop=False)
                                P.mm(oc, SM8[:, buf, c, :], QT[:, h, c * 64:(c + 1) * 64], start=False, stop=True)

                        def hg_hn(h):
                            headnorm_out(OH[h % 2], GG[:, h, :], PHN[:, l, 0, h:h + 1], MIX[:, h, :], bank=6)

                        hg_pp(0); hg_pp(1); hg_chain(0); hg_chain(1); hg_o(0); hg_o(1)
                        hg_pp(2); hg_pp(3); hg_chain(2); hg_chain(3); hg_hn(0); hg_hn(1)
                        hg_o(2); hg_o(3); hg_hn(2); hg_hn(3)
                        if l == 0 and ti == 0:
                            tap("hg_out", MIX[:, 0:4, :])
                        mark("wout_h")
                        if "ssd" not in subs:
                            wout_partial(l, 0, 4)

                    if "ret" in subs:
                        mark("ret_proj")
                        ROT = FT[:, 2:4, 0:TT]
                        QR = ibc(1, 4).rearrange("p (h t) -> p h t", t=TT)
                        QH = ibc(5, 4).rearrange("p (h t) -> p h t", t=TT)
                        KR = IT(0, 2048).rearrange("p (h t) -> p h t", t=TT)
                        VR = IT(2048, 4096).rearrange("p (b n) -> p b n", n=512)
                        P.dma(ROT[:, 0, :], cin["rot_cos"][:, t0:t0 + TT])
                        P.dma(ROT[:, 1, :], cin["rot_sin"][:, t0:t0 + TT])
                        for (cq, dst, isq) in ((2048, QR, True), (2560, KR, False)):
                            for h, ps in fm_slice("w_in", l, cq, 4, XN):
                                xb = ibc(9)
                                P.copy(xb, ps)
                                ps2 = PF[:, 3, :]
                                P.mm(ps2, c_prot_bf[:], xb)
                                t1 = FT[:, 0, 0:TT]
                                P.tt(t1, ps, ROT[:, 0, :], MUL)
                                t2 = FT[:, 1, 0:TT]
                                P.tt(t2, ps2, ROT[:, 1, :], MUL)
                                P.tt(dst[:, h, :], t1, t2, ADD, eng="pool")
                                if isq:
                                    P.tt(QH[:, h, :].rearrange("p (c n) -> p c n", n=128),
                                         dst[:, h, :].rearrange("p (c n) -> p c n", n=128),
                                         c_grow[:, h, :].unsqueeze(1).to_broadcast([128, 4, 128]), MUL)
                        cut(1)
                        for bk, b0, ps in tm_slice("w_in", l, 3072, 512, 128, 4):
                            P.copy(VR[:, bk, b0:b0 + 256], ps)
                        for h, ps in fm_slice("w_in", l, 3584, 4, XN):
                            P.act(GG[:, h, :], ps, AF.Silu)
                        cut(2)
                        mark("ret_batch")
                        KHM = IT(4096, 6144).rearrange("p (h c n) -> p h c n", h=4, c=4)
                        AR = IT(6144, 8192).rearrange("p (h c n) -> p h c n", h=4, c=4)
                        for h in range(4):
                            pb = PB[:, 0:512].rearrange("p (c n) -> p c n", n=128)
                            for c in range(4):
                                P.tr(pb[:, c, :], KR[:, h, c * 128:(c + 1) * 128], c_ident_bf[:])
                            P.act(KHM[:, h, :, :], pb, AF.Identity, scale=c_kscale[:, h:h + 1])
                            pa = PF[:, 6, :].rearrange("p (c n) -> p c n", n=128)
                            for c in range(4):
                                P.mm(pa[:, c, :], KR[:, h, c * 128:(c + 1) * 128], QR[:, h, c * 128:(c + 1) * 128])
                            P.tt(AR[:, h, :, :], pa, c_dmask[:, h, :].unsqueeze(1).to_broadcast([128, 4, 128]), MUL)
                        cut(3)
                        mark("ret_rec")
                        SM4 = ST2[:, 0:1024].rearrange("p (b c n) -> p b c n", b=2, c=4)

                        def rt_pp(h):
                            buf = h % 2
                            for c in range(4):
                                P.mm(PF[:, 2 * buf, c * 128:(c + 1) * 128], KHM[:, h, c, :], VR[:, c, h * 128:(h + 1) * 128])
                            dst = ST[:, 0:512] if buf == 0 else FT[:, 0, 0:512]
                            P.copy(dst, PF[:, 2 * buf, :])

                        def rt_chain(h):
                            buf = h % 2
                            for c in range(4):
                                pps = ST[:, c * 128:(c + 1) * 128] if buf == 0 else FT[:, 0, c * 128:(c + 1) * 128]
                                P.copy(SM4[:, buf, c, :], RS[:, h, :], eng="dve")
                                P.stt(RS[:, h, :], RS[:, h, :], hc["ret_g128"][h], pps, MUL, ADD)

                        def rt_o(h):
                            buf = h % 2
                            for c in range(4):
                                oc = OH[buf][:, c * 128:(c + 1) * 128]
                                P.mm(oc, VR[:, c, h * 128:(h + 1) * 128], AR[:, h, c, :], start=True, stop=False)
                                P.mm(oc, SM4[:, buf, c, :], QH[:, h, c * 128:(c + 1) * 128], start=False, stop=True)

                        def rt_hn(h):
                            headnorm_out(OH[h % 2], GG[:, h, :], PHN[:, l, 1, h:h + 1], MIX[:, 4 + h, :], bank=6)

                        rt_pp(0); rt_pp(1); rt_chain(0); rt_chain(1); rt_o(0); rt_o(1)
                        rt_pp(2); rt_pp(3); rt_chain(2); rt_chain(3); rt_hn(0); rt_hn(1)
                        rt_o(2); rt_o(3); rt_hn(2); rt_hn(3)
                        P.mute = False
                        if l == 0 and ti == 0:
                            tap("ret_out", MIX[:, 4:8, :])
                        mark("wout_r")
                        if "ssd" not in subs:
                            wout_partial(l, 4, 4)

                    if "ssd" in subs:
                        mark("ssd_proj")
                        ZS = IT(0, 4096).rearrange("p (b n) -> p b n", n=1024)
                        for bk, b0, ps in tm_slice("w_in", l, 4096, 1024, 128, 4):
                            P.act(ZS[:, bk, b0:b0 + 256], ps, AF.Silu)
                        XBC = ibc(1, 8).rearrange("p (c t) -> p c t", t=TT)
                        BCm = BCv
                        if True:
                            for ch, ps in fm_slice("w_in", l, 5120, 16, XN):
                                U = FT[:, 2 * (ch % 2), 0:TT + 3]
                                P.copy(U[:, 0:3], CS[:, ch, :], eng="pool")
                                P.copy(U[:, 3:TT + 3], ps)
                                acc = FT[:, 2 * (ch % 2) + 1, 0:TT]
                                P.act(acc, ps, AF.Identity, bias=PCW[:, l, ch, 4:5], scale=PCW[:, l, ch, 3:4])
                                for j in range(3):
                                    P.stt(acc, U[:, j:j + TT], PCW[:, l, ch, j:j + 1], acc, MUL, ADD)
                                P.copy(CS[:, ch, :], U[:, TT:TT + 3], eng="pool")
                                dst = XBC[:, ch, :] if ch < 8 else BCm[:, ch - 8, :]
                                P.act(dst, acc, AF.Silu)
                        wv = wload("w_in", l, 7168, 16)
                        pdt = PF[:, 3, 0:64].rearrange("p (b n) -> p b n", n=16)
                        for bk in range(4):
                            for k in range(NCH):
                                P.mm(pdt[:, bk, :], XN[:, k, bk * 128:(bk + 1) * 128], wv[:, k, :], start=(k == 0), stop=(k == NCH - 1))
                        DTS = SM[:, 96:160].rearrange("p (b n) -> p b n", n=16)
                        LA = SM[:, 160:224].rearrange("p (b n) -> p b n", n=16)
                        P.tt(DTS, pdt, PROW[:, l, 0, :].unsqueeze(1).to_broadcast([128, 4, 16]), ADD)
                        P.act(DTS, DTS, AF.Exp)
                        P.act(DTS, DTS, AF.Ln, bias=1.0)
                        P.tt(LA, DTS, PROW[:, l, 1, :].unsqueeze(1).to_broadcast([128, 4, 16]), MUL)
                        mark("ssd_loop")
                        SSB = SBF[:, 0:1024]
                        VHM = SBF[:, 1024:2048]
                        XTM = ST2[:, 0:1024]
                        VTM = ST2[:, 1024:2048]
                        BTM = IT(4096, 4608)
                        GS = IT(4608, 5120).rearrange("p (g n) -> p g n", n=128)
                        LTs = [IT(5120, 5632).rearrange("p (h n) -> p h n", n=128), IT(6144, 6656).rearrange("p (h n) -> p h n", n=128)]
                        MTs = [IT(5632, 6144).rearrange("p (h n) -> p h n", n=128), IT(6656, 7168).rearrange("p (h n) -> p h n", n=128)]
                        YA = [PF[:, 4, :], PF[:, 5, :]]
                        YB = [PF[:, 0, :], PF[:, 1, :]]
                        PSS = [PF[:, 2, :], PF[:, 3, :]]
                        h64 = lambda ap: ap.rearrange("p (h n) -> p h n", n=64)
                        for c in range(4):
                            tk = slice(c * 128, (c + 1) * 128)
                            psm = PF[:, 6, 0:48].rearrange("p (a n) -> p a n", n=16)
                            P.mm(psm[:, 0, :], c_tri_f[:], LA[:, c, :])
                            P.mm(psm[:, 1, :], c_strict_f[:], LA[:, c, :])
                            P.mm(psm[:, 2, :], c_ones_f[:], LA[:, c, :])
                            ECt = SM[:, 224:256].rearrange("p (a n) -> p a n", n=16) if False else FT[:, 5, 0:48].rearrange("p (a n) -> p a n", n=16)
                            P.act(ECt, psm, AF.Exp)
                            pb = PB[:, :].rearrange("p (c n) -> p c n", n=128)
                            for j in range(8):
                                P.tr(pb[:, j, :], XBC[:, j, tk], c_ident_bf[:])
                            P.copy(XTM, PB[:, :])
                            P.tt(h64(VTM), h64(XTM), DTS[:, c, :].unsqueeze(2).to_broadcast([128, 16, 64]), MUL)
                            P.tt(h64(VHM), h64(VTM), ECt[:, 1, :].unsqueeze(2).to_broadcast([128, 16, 64]), MUL)
                            pb2 = PB[:, 0:512].rearrange("p (c n) -> p c n", n=128)
                            for g in range(4):
                                P.tr(pb2[:, g, :], BCm[:, g, tk], c_ident_bf[:])
                            P.copy(BTM, PB[:, 0:512])
                            pg = PF[:, 6, :].rearrange("p (g n) -> p g n", n=128)
                            for g in range(4):
                                P.mm(pg[:, g, :], BCm[:, g, tk], BCm[:, 4 + g, tk])
                            P.copy(GS, pg)
                            def seg_stage(g):
                                segL = ST[:, (g % 2) * 512:(g % 2 + 1) * 512].rearrange("p (h n) -> p h n", n=128)
                                P.tt(segL, c_strict_f[:].unsqueeze(1).to_broadcast([128, 4, 128]),
                                     LA[:, c, 4 * g:4 * g + 4].unsqueeze(2).to_broadcast([128, 4, 128]), MUL)
                                pseg = PF[:, 6 if g % 2 == 0 else 3, :]
                                P.mm(pseg, c_ident_bf[:], c_neg4_bf[:], start=True, stop=False)
                                for hh in range(4):
                                    P.mm(pseg[:, hh * 128:(hh + 1) * 128], segL[:, hh, :], c_tri_f[:], start=False, stop=(hh == 3))

                            def out_stage(g):
                                LT, MT = LTs[g % 2], MTs[g % 2]
                                pseg = PF[:, 6 if g % 2 == 0 else 3, :]
                                P.act(LT, pseg.rearrange("p (h n) -> p h n", n=128), AF.Exp)
                                P.tt(MT, LT, GS[:, g, :].unsqueeze(1).to_broadcast([128, 4, 128]), MUL)
                                for hh in range(4):
                                    hd = 4 * g + hh
                                    P.mm(YA[hd // 8][:, (hd % 8) * 64:(hd % 8) * 64 + 64], MT[:, hh, :], VTM[:, hd * 64:(hd + 1) * 64])

                            seg_stage(0); seg_stage(1); out_stage(0); seg_stage(2); out_stage(1)
                            seg_stage(3); out_stage(2); out_stage(3)
                            P.copy(SSB, SS[:])
                            for g in range(4):
                                P.mm(YB[g // 2][:, (g % 2) * 256:(g % 2) * 256 + 256], BCm[:, 4 + g, tk], SSB[:, g * 256:(g + 1) * 256])
                            for g in range(4):
                                P.mm(PSS[g // 2][:, (g % 2) * 256:(g % 2) * 256 + 256], BTM[:, g * 128:(g + 1) * 128], VHM[:, g * 256:(g + 1) * 256])
                            y = ST[:, :]
                            for hf in range(2):
                                ysl = y[:, hf * 512:(hf + 1) * 512]
                                P.tt(h64(ysl), h64(YB[hf]), ECt[:, 0, hf * 8:hf * 8 + 8].unsqueeze(2).to_broadcast([128, 8, 64]), MUL)
                                P.tt(ysl, YA[hf], ysl, ADD)
                                t3 = FT[:, hf, 0:512]
                                P.tt(h64(t3), h64(XTM[:, hf * 512:(hf + 1) * 512]),
                                     PROW[:, l, 2, hf * 8:hf * 8 + 8].unsqueeze(2).to_broadcast([128, 8, 64]), MUL)
                                P.tt(ysl, ysl, t3, ADD, eng="pool")
                            P.tt(y, y, ZS[:, c, :], MUL)
                            ssq = SM[:, 24:28]
                            junk = FT[:, 2, 0:256]
                            for g in range(4):
                                P.act(junk, y[:, g * 256:(g + 1) * 256], AF.Square, accum_out=ssq[:, g:g + 1])
                            rs = SM[:, 28:32]
                            P.act(rs, ssq, AF.Sqrt, bias=EPS, scale=1.0 / 256)
                            P.recip(rs, rs)
                            y4 = y.rearrange("p (g n) -> p g n", n=256)
                            P.tt(y4, y4, rs.unsqueeze(2).to_broadcast([128, 4, 256]), MUL)
                            YN = ST2[:, 0:1024]
                            P.tt(YN, y, PSN[:], MUL)
                            pbo = PB[:, :].rearrange("p (c n) -> p c n", n=128)
                            for j in range(8):
                                P.tr(pbo[:, j, :], YN[:, j * 128:(j + 1) * 128], c_ident_bf[:])
                            P.copy(MIX[:, 8:16, tk], pbo)
                            P.tt(h64(SS[:]), h64(SS[:]), ECt[:, 2, :].unsqueeze(2).to_broadcast([128, 16, 64]), MUL)
                            for hf in range(2):
                                P.tt(SS[:, hf * 512:(hf + 1) * 512], SS[:, hf * 512:(hf + 1) * 512], PSS[hf], ADD)
                            if c == 0 and "hgrn" in subs:
                                wout_partial(l, 0, 4)
                            if c == 1 and "ret" in subs:
                                wout_partial(l, 4, 4)
                        if l == 0 and ti == 0:
                            tap("ssm_out", MIX[:, 8:16, :])
                        mark("wout_s")
                        wout_partial(l, 8, 8)
                    if l == 0 and ti == 0:
                        tap("x1", X[:])

                if "xattn" in phases:
                    mark("xattn")
                    rmsnorm_fm(X[:], XN[:], PV[:, l, 1, :], TT)
                    QA = ibc(1, 4).rearrange("p (h t) -> p h t", t=TT)
                    AO = ibc(5, 4).rearrange("p (h t) -> p h t", t=TT)
                    for h, ps in fm_slice("xa_wq", l, 0, 4, XN):
                        P.copy(QA[:, h, :], ps)
                    for h in range(4):
                        ET = IT(h * 1024, (h + 1) * 1024).rearrange("p (m t) -> p m t", t=TT)
                        for mb in range(2):
                            pss = PF[:, 4 + mb, :]
                            P.mm(pss, KT[:, h, mb * 128:(mb + 1) * 128], QA[:, h, :])
                            P.act(ET[:, mb, :], pss, AF.Exp, scale=128 ** -0.5)
                        den = PF[:, 6 if h % 2 == 0 else 3, :]
                        P.mm(den, c_ones_bf[:], ET[:, 0, :], start=True, stop=False)
                        P.mm(den, c_ones_bf[:], ET[:, 1, :], start=False, stop=True)
                        rden = FT[:, h, 0:TT]
                        P.recip(rden, den)
                        po = PF[:, nb(), :]
                        P.mm(po, VV[:, 0, h * 128:(h + 1) * 128], ET[:, 0, :], start=True, stop=False)
                        P.mm(po, VV[:, 1, h * 128:(h + 1) * 128], ET[:, 1, :], start=False, stop=True)
                        P.tt(AO[:, h, :], po, rden, MUL)
                    for m, ps in fm_slice("xa_wo", l, 0, NCH, AO, KC=4, cpb=8):
                        P.tt(X[:, m, :], X[:, m, :], ps, ADD)
                    if l == 0 and ti == 0:
                        tap("x2", X[:])

                if "ffn" in phases:
                    mark("ffn")
                    rmsnorm_fm(X[:], XN[:], PV[:, l, 3, :], TT)
                    for (j0, nj) in FFN_PARTS:
                        if True:
                            for (jj, psg), (_, psu) in zip(fm_slice("ffn_w_up", l, j0 * 128, nj, XN),
                                                           fm_slice("ffn_w_up", l, FFN + j0 * 128, nj, XN)):
                                j = j0 + jj
                                res = []
                                for (ps, cidx, slot) in ((psg, j, 0), (psu, 44 + j, 1)):
                                    U = FT[:, slot * 2, 0:TT + 2]
                                    P.copy(U[:, 0:2], CF[:, cidx, :], eng="pool")
                                    P.copy(U[:, 2:TT + 2], ps)
                                    acc = FT[:, slot * 2 + 1, 0:TT]
                                    P.act(acc, ps, AF.Identity, bias=PFW[:, cidx, 3:4], scale=PFW[:, cidx, 2:3])
                                    for q in range(2):
                                        P.stt(acc, U[:, q:q + TT], PFW[:, cidx, q:q + 1], acc, MUL, ADD)
                                    P.copy(CF[:, cidx, :], U[:, TT:TT + 2], eng="pool")
                                    res.append(acc)
                                sg = FT[:, 4, 0:TT]
                                P.act(sg, res[0], AF.Silu)
                                P.tt(HID[:, j - j0, :], sg, res[1], MUL, eng="pool")
                        for m, ps in fm_slice("ffn_w_down", l, 0, NCH, HID, KC=nj, r0=j0):
                            P.tt(X[:, m, :], X[:, m, :], ps, ADD)
                            if (not last) and j0 == FFN_PARTS[-1][0]:
                                P.dma(xs_d[ti, :, m, :], X[:, m, :])
                    if l == 0 and ti == 0:
                        tap("x3", X[:])

                mark("store")
                if last:
                    rstd = rmsnorm_fm(X[:], None, None, TT)
                    for k in range(NCH):
                        P.stt(X[:, k, :], X[:, k, :], PFIN[:, k:k + 1], rstd, MUL, MUL)
                    P.dma(y_out[ti, :, 0:8, :], X[:, 0:8, :])
                    P.dma(y_out[ti, :, 8:16, :], X[:, 8:16, :])
                elif "ffn" not in phases:
                    P.dma(xs_d[ti, :, 0:8, :], X[:, 0:8, :])
                    P.dma(xs_d[ti, :, 8:16, :], X[:, 8:16, :])

        final = P.final_wait_all()
        with nc.Block() as block:
            @block.tensor
            def _(e):
                P.replay("pe", e)

            @block.scalar
            def _(e):
                P.replay("act", e)

            @block.vector
            def _(e):
                P.replay("dve", e)

            @block.gpsimd
            def _(e):
                P.replay("pool", e)

            @block.sync
            def _(e):
                P.replay("sp", e)
                for s, v in final:
                    e.wait_ge(s, v)
        if len(phases) == 3:
            assert wstate["next"] == WE, (wstate["next"], WE)
        build.n_ops = P.n_ops
        build.counts = {e: len(P.ops[e]) for e in P.ENGS}
    return nc


def _fm(v):
    v = np.asarray(v, np.float32)
    lead = v.shape[:-1]
    return np.ascontiguousarray(np.moveaxis(v.reshape(lead + (-1, 128)), -1, 0))


def prep_shared(inp, L, T, wreg):
    hc = host_constants(T)
    sh = {}
    ws = np.zeros((L, 128, WE), np.float32)
    for (wname, r0, KC, c0, ncols), off in wreg.items():
        W = np.asarray(inp[wname], np.float32)
        for l in range(L):
            blk = W[l, r0 * 128:(r0 + KC) * 128, c0:c0 + ncols].reshape(KC, 128, ncols)
            ws[l, :, off:off + KC * ncols] = blk.transpose(1, 0, 2).reshape(128, KC * ncols)
    sh["wstream"] = ws
    pvec = np.stack([_fm(inp["norm_mix"][:L]), _fm(inp["norm_xattn"][:L]), _fm(inp["norm_mem"][:L]),
                     _fm(inp["norm_ffn"][:L])], axis=2)
    sh["pvec"] = np.ascontiguousarray(pvec)
    sh["pfin"] = _fm(inp["norm_final"])
    sh["plb"] = _fm(inp["hg_lb_logits"])
    sh["phn"] = np.ascontiguousarray(np.stack([_fm(inp["hg_norm"][:L]), _fm(inp["ret_norm"][:L])], axis=2))
    cw = _fm(np.asarray(inp["ssm_conv_w"], np.float32)[:L])
    cb = _fm(np.asarray(inp["ssm_conv_b"], np.float32)[:L])
    sh["pcw"] = np.ascontiguousarray(np.concatenate([np.moveaxis(cw, 2, 3), cb[..., None]], axis=3))
    sh["prow"] = np.ascontiguousarray(np.stack([inp["ssm_dt_bias"][:L], inp["ssm_A_log"][:L], inp["ssm_D"][:L]], axis=1).astype(np.float32))
    sh["psn"] = np.ascontiguousarray(np.asarray(inp["ssm_norm"], np.float32)[:L])
    fw = _fm(np.asarray(inp["ffn_conv_w"], np.float32)[:L])
    fb = _fm(np.asarray(inp["ffn_conv_b"], np.float32)[:L])
    sh["pfw"] = np.ascontiguousarray(np.moveaxis(np.concatenate([np.moveaxis(fw, 2, 3), fb[..., None]], axis=3), 1, 0))
    for k in ("ident_f", "ones_f", "tri_f", "strict_f", "neg4", "causal64", "ret_dmask", "ret_grow",
              "ret_kscale", "protT", "rot_cos", "rot_sin", "resetmask"):
        sh["k_" + k] = np.ascontiguousarray(hc[k])
    return sh


def tile_x(xb, T):
    NT = T // TT
    a = np.asarray(xb, np.float32).reshape(NT, TT, NCH, 128)
    return np.ascontiguousarray(a.transpose(0, 3, 2, 1))


def untile_y(yt, T):
    NT = T // TT
    return np.ascontiguousarray(yt.transpose(0, 3, 2, 1)).reshape(T, D)


def kernel(**inp):
    B = inp["x"].shape[0]
    T = inp["x"].shape[1]
    L = inp["w_in"].shape[0]
    nc = build(L, T)
    sh = prep_shared(inp, L, T, build.wreg)
    in_maps = []
    for b in range(B):
        m = dict(sh)
        m["x_t"] = tile_x(inp["x"][b], T)
        m["mem_t"] = _fm(inp["mem"][b]).reshape(128, NMEM, NCH).transpose(0, 2, 1).copy()
        in_maps.append(m)
    res = run_bass_kernel_spmd(nc, in_maps, core_ids=list(range(B)))
    out = np.stack([untile_y(np.asarray(r["y_t"]), T) for r in res.results], axis=0)
    return out.astype(np.float32)
```

```python
import math
import os
import contextlib
import numpy as np
import concourse.bass as bass
import concourse.mybir as mybir
from concourse.bass_utils import run_bass_kernel_spmd

F32 = mybir.dt.float32
BF16 = mybir.dt.bfloat16
AF = mybir.ActivationFunctionType
ALU = mybir.AluOpType

D = 2048
NCH = 16
TT = 512
L_FULL = 4
T_FULL = 4096
NMEM = 256
IN_COLS = 7184
FFN = 5632
EPS = 1e-6
FFN_PARTS = [(0, 11), (11, 11), (22, 11), (33, 11)]
NSLOT = 4
WE = (D * IN_COLS + D * D + D * 512 + D * 1024 + 512 * D + D * 2 * FFN + FFN * D) // 128

SAME_ENGINE_SYNC = os.environ.get("SES", "1") == "1"
EPOCH = 30000
N_DMA_SEMS = 40


class Prog:
    ENGS = ("pe", "act", "dve", "pool", "sp")

    def __init__(self, nc, stack):
        self.nc = nc
        self.stack = stack
        self.ops = {e: [] for e in self.ENGS}
        self.nsem = 0
        self.eng_sem = {e: None for e in self.ENGS}
        self.eng_cnt = {e: 0 for e in self.ENGS}
        self.known = {e: {} for e in self.ENGS}
        self.segs = {}
        self.dma_sems = [self._new_sem("dq%d" % i) for i in range(N_DMA_SEMS)]
        self.dma_cnt = [0] * N_DMA_SEMS
        self.dma_rr = 0
        self.dma_rr_pool = 0
        self.pool_depth = 12
        self.semobj = {}
        self.n_ops = 0
        self.mute = False

    def _new_sem(self, name):
        s = self.stack.enter_context(self.nc.semaphore(name))
        self.nsem += 1
        return s

    colspace = {}

    @classmethod
    def region(cls, ap):
        steps = ap.ap
        off = ap.offset
        space = str(ap.space)
        esz = 2 if ap.dtype == BF16 else 4
        E = cls.colspace.get(ap.tensor.name)
        if E is not None:
            lay, rem = divmod(off, 128 * E)
            col = rem % E
            return ("%s:%d" % (ap.tensor.name, lay), col * esz, (col + steps[-1][1]) * esz)
        if space in ("SB", "PSUM"):
            pstride = steps[0][0]
            lo = off % pstride if pstride > 0 else off
            ext = 1
            for st, cnt in steps[1:]:
                ext += (cnt - 1) * abs(st)
            return (ap.tensor.name, lo * esz, (lo + ext) * esz)
        ext = 1
        for st, cnt in steps:
            ext += (cnt - 1) * abs(st)
        return (ap.tensor.name, off * esz, (off + ext) * esz)

    @classmethod
    def psum_bank_region(cls, ap):
        name, lo, hi = cls.region(ap)
        b0 = lo // 2048
        b1 = (hi - 1) // 2048
        return (name, b0 * 2048, (b1 + 1) * 2048)

    def _collect(self, reg, is_write, deps):
        name, lo, hi = reg
        for s in self.segs.get(name, ()):
            if s[0] < hi and lo < s[1]:
                w = s[2]
                if w is not None:
                    self._add(deps, w)
                if is_write:
                    for r in s[3].values():
                        self._add(deps, r)

    @staticmethod
    def _add(deps, tok):
        k = id(tok[0])
        o = deps.get(k)
        if o is None or o[1] < tok[1]:
            deps[k] = tok

    def _commit(self, reg, is_write, tok):
        name, lo, hi = reg
        segs = self.segs.setdefault(name, [])
        out = []
        covered = []
        for s in segs:
            if s[1] <= lo or s[0] >= hi:
                out.append(s)
                continue
            if s[0] < lo:
                out.append([s[0], lo, s[2], dict(s[3])])
            if s[1] > hi:
                out.append([hi, s[1], s[2], dict(s[3])])
            a, b = max(s[0], lo), min(s[1], hi)
            covered.append((a, b))
            if is_write:
                pass
            else:
                r = dict(s[3])
                k = id(tok[0])
                o = r.get(k)
                if o is None or o[1] < tok[1]:
                    r[k] = tok
                out.append([a, b, s[2], r])
        if is_write:
            out.append([lo, hi, tok, {}])
        else:
            covered.sort()
            cur = lo
            for a, b in covered:
                if a > cur:
                    out.append([cur, a, None, {id(tok[0]): tok}])
                cur = max(cur, b)
            if cur < hi:
                out.append([cur, hi, None, {id(tok[0]): tok}])
        self.segs[name] = out

    def _next_token(self, eng):
        if self.eng_sem[eng] is None or self.eng_cnt[eng] >= EPOCH:
            self.eng_sem[eng] = self._new_sem("e_%s_%d" % (eng, self.nsem))
            self.eng_cnt[eng] = 0
        self.eng_cnt[eng] += 1
        return (self.eng_sem[eng], self.eng_cnt[eng], eng)

    def op(self, eng, fn, reads=(), writes=(), dma=False):
        if self.mute:
            return
        self.n_ops += 1
        deps = {}
        rregs = []
        wregs = []
        for a in reads:
            if str(a.space) == "PSUM":
                wregs.append(self.psum_bank_region(a))
            else:
                rregs.append(self.region(a))
        for a in writes:
            if str(a.space) == "PSUM":
                wregs.append(self.psum_bank_region(a))
            else:
                wregs.append(self.region(a))
        for r in rregs:
            self._collect(r, False, deps)
        for w in wregs:
            self._collect(w, True, deps)
        if dma:
            if eng == "pool":
                base = N_DMA_SEMS - 12
                i = base + self.dma_rr_pool % self.pool_depth
                self.dma_rr_pool += 1
            else:
                i = self.dma_rr
                self.dma_rr = (self.dma_rr + 1) % (N_DMA_SEMS - 12)
            sem = self.dma_sems[i]
            if self.dma_cnt[i] > 0:
                self._add(deps, (sem, self.dma_cnt[i], "dma"))
            self.dma_cnt[i] += 16
            tok = (sem, self.dma_cnt[i], "dma")
            inc = 16
        else:
            tok = self._next_token(eng)
            inc = 1
        waits = []
        kn = self.known[eng]
        for t in deps.values():
            if t[2] == eng:
                if eng == "pe" or not SAME_ENGINE_SYNC:
                    continue
            k = id(t[0])
            if kn.get(k, 0) >= t[1]:
                continue
            kn[k] = t[1]
            waits.append((t[0], t[1]))
        for r in rregs:
            self._commit(r, False, tok)
        for w in wregs:
            self._commit(w, True, tok)
        self.ops[eng].append((waits, fn, tok[0], inc))

    def replay(self, eng, handle):
        for waits, fn, sem, inc in self.ops[eng]:
            for s, v in waits:
                handle.wait_ge(s, v)
            ins = fn(handle)
            ins.then_inc(sem, inc)

    def final_wait_all(self, eng_handle_name="sp"):
        waits = []
        for e in self.ENGS:
            if self.eng_sem[e] is not None:
                waits.append((self.eng_sem[e], self.eng_cnt[e]))
        for i, s in enumerate(self.dma_sems):
            if self.dma_cnt[i] > 0:
                waits.append((s, self.dma_cnt[i]))
        return waits

    def mm(self, out, lhsT, rhs, start=True, stop=True):
        self.op("pe", lambda e: e.matmul(out, lhsT, rhs, start=start, stop=stop),
                reads=[lhsT, rhs], writes=[out])

    def tr(self, out, in_, ident):
        self.op("pe", lambda e: e.transpose(out, in_, ident), reads=[in_, ident], writes=[out])

    def act(self, out, in_, func, bias=None, scale=None, accum_out=None, eng="act"):
        reads = [in_]
        kw = {}
        if bias is not None:
            kw["bias"] = bias
            if not isinstance(bias, (int, float)):
                reads.append(bias)
        if scale is not None:
            kw["scale"] = scale
            if not isinstance(scale, (int, float)):
                reads.append(scale)
        writes = [out]
        if accum_out is not None:
            kw["accum_out"] = accum_out
            writes.append(accum_out)
        self.op("act", lambda e: e.activation(out, in_, func, **kw), reads=reads, writes=writes)

    def tt(self, out, in0, in1, op, eng="dve"):
        self.op(eng, lambda e: e.tensor_tensor(out, in0, in1, op), reads=[in0, in1], writes=[out])

    def ts(self, out, in0, s1, op0, s2=None, op1=None, eng="dve"):
        reads = [in0]
        if not isinstance(s1, (int, float)):
            reads.append(s1)
        if s2 is not None and not isinstance(s2, (int, float)):
            reads.append(s2)
        if op1 is None:
            self.op(eng, lambda e: e.tensor_scalar(out, in0, s1, None, op0), reads=reads, writes=[out])
        else:
            self.op(eng, lambda e: e.tensor_scalar(out, in0, s1, s2, op0, op1), reads=reads, writes=[out])

    def stt(self, out, in0, scalar, in1, op0, op1):
        reads = [in0, in1]
        if not isinstance(scalar, (int, float)):
            reads.append(scalar)
        self.op("dve", lambda e: e.scalar_tensor_tensor(out, in0, scalar, in1, op0, op1),
                reads=reads, writes=[out])

    def copy(self, out, in_, eng="act"):
        if eng == "act":
            self.op("act", lambda e: e.copy(out, in_), reads=[in_], writes=[out])
        else:
            self.op(eng, lambda e: e.tensor_copy(out, in_), reads=[in_], writes=[out])

    def memset(self, ap, val, eng="dve"):
        self.op(eng, lambda e: e.memset(ap, val), reads=[], writes=[ap])

    def recip(self, out, in_):
        self.op("dve", lambda e: e.reciprocal(out, in_), reads=[in_], writes=[out])

    def scan(self, out, d0, d1, initial, op0, op1):
        self.op("dve", lambda e: e.tensor_tensor_scan(out, d0, d1, initial, op0, op1),
                reads=[d0, d1], writes=[out])

    def dma(self, out, in_, eng="sp"):
        self.op(eng, lambda e: e.dma_start(out=out, in_=in_), reads=[in_], writes=[out], dma=True)


def host_constants(T):
    c = {}
    i = np.arange(128)
    c["ident_bf"] = np.eye(128, dtype=np.float32)
    c["ident_f"] = np.eye(128, dtype=np.float32)
    c["ones_f"] = np.ones((128, 128), np.float32)
    c["tri_f"] = (i[:, None] <= i[None, :]).astype(np.float32)
    c["strict_f"] = (i[:, None] > i[None, :]).astype(np.float32)
    neg = np.where(i[None, :] < i[:, None], -30000.0, 0.0).astype(np.float32)
    c["neg4"] = np.tile(neg, (1, 4))
    j = np.arange(64)
    c["causal64"] = (j[None, :] >= j[:, None]).astype(np.float32)
    gam = 1.0 - 2.0 ** (-5.0 - np.arange(4, dtype=np.float64))
    lg = np.log(gam.astype(np.float32)).astype(np.float32).astype(np.float64)
    dm = np.zeros((128, 4, 128), np.float64)
    for h in range(4):
        dm[:, h, :] = np.where(i[None, :] >= i[:, None], np.exp(lg[h] * (i[None, :] - i[:, None])), 0.0)
    c["ret_dmask"] = (dm * 128 ** -0.5).astype(np.float32)
    c["ret_grow"] = np.broadcast_to(np.exp(lg[None, :, None] * (i[None, None, :] + 1.0)), (128, 4, 128)).astype(np.float32)
    c["ret_kscale"] = (np.exp(lg[None, :] * (127.0 - i[:, None])) * 128 ** -0.5).astype(np.float32)
    c["ret_g128"] = [float(np.exp(lg[h] * 128.0)) for h in range(4)]
    prot = np.zeros((128, 128), np.float32)
    for m in range(128):
        prot[(m + 64) % 128, m] = 1.0
    c["protT"] = prot
    half = 64
    inv = (10000.0 ** (-np.arange(half, dtype=np.float32) / half)).astype(np.float32)
    ang = np.arange(T, dtype=np.float32)[:, None] * inv[None, :]
    cos = np.cos(ang).astype(np.float32).T
    sin = np.sin(ang).astype(np.float32).T
    c["rot_cos"] = np.concatenate([cos, cos], 0)
    c["rot_sin"] = np.concatenate([-sin, sin], 0)
    rm = np.ones((128, TT), np.float32)
    rm[:, ::64] = 0.0
    c["resetmask"] = rm
    return c


def build(L=L_FULL, T=T_FULL, taps=None, phases=("mixer", "xattn", "ffn")):
    NT = T // TT
    nc = bass.Bass("TRN2", target_bir_lowering=False)
    hc = host_constants(T)
    taps = taps or ()
    subs = ("hgrn", "ret", "ssd") if "mixer" in phases else tuple(p for p in phases if p in ("hgrn", "ret", "ssd"))

    def din(name, shape, dt=F32):
        return nc.dram_tensor(name, list(shape), dt, kind="ExternalInput").ap()

    x_in = din("x_t", [NT, 128, NCH, TT])
    mem_in = din("mem_t", [128, NCH, NMEM])
    wsf = din("wstream", [L, 128, WE])
    pv = din("pvec", [128, L, 4, NCH])
    pfin = din("pfin", [128, NCH])
    plb = din("plb", [128, L_FULL, 4])
    phn = din("phn", [128, L, 2, 4])
    pcw = din("pcw", [128, L, NCH, 5])
    prow = din("prow", [L, 3, 16])
    psn = din("psn", [L, 1024])
    pfw = din("pfw", [L, 128, 88, 4])
    cin = {}
    for k in ("ident_f", "ones_f", "tri_f", "strict_f", "neg4", "causal64", "ret_dmask", "ret_grow",
              "ret_kscale", "protT", "rot_cos", "rot_sin", "resetmask"):
        cin[k] = din("k_" + k, hc[k].shape)
    y_out = nc.dram_tensor("y_t", [NT, 128, NCH, TT], F32, kind="ExternalOutput").ap()
    tap_out = {}

    def dint(name, shape, dt):
        return nc.dram_tensor(name, list(shape), dt, kind="Internal").ap()

    xs_d = dint("xs_d", [NT, 128, NCH, TT], F32)
    wsb = [dint("wstream_b%d" % l, [128, WE], BF16) for l in range(L)]
    Prog.colspace = {"wstream_b%d" % l: WE for l in range(L)}

    stack = contextlib.ExitStack()
    with stack:
        P = Prog(nc, stack)

        def sb(name, shape, dt=F32):
            return stack.enter_context(nc.sbuf_tensor(name, list(shape), dt))

        X = sb("X", [128, NCH, TT])
        XN = sb("XN", [128, NCH, TT], BF16)
        MIX = sb("MIX", [128, 8, TT], BF16)
        BIG = sb("BIG", [128, 36 * TT], BF16)
        WS = sb("WS", [128, NSLOT, 4096], BF16)
        FT = sb("FT", [128, 6, TT + 4])
        ST = sb("ST", [128, 1024])
        ST2 = sb("ST2", [128, 2048], BF16)
        SBF = sb("SBF", [128, 2048], BF16)
        SMB = sb("SMB", [128, 4, 128], BF16)
        HR = sb("HR", [128, 4, 128])
        RS = sb("RS", [128, 4, 128])
        SS = sb("SS", [128, 1024])
        KT = sb("KT", [128, 4, NMEM], BF16)
        VV = sb("VV", [128, 2, 512], BF16)
        CF = sb("CF", [128, 88, 2])
        CS = sb("CS", [128, NCH, 3])
        SM = sb("SM", [128, 256])
        c_ident_bf = sb("c_ident_bf", [128, 128], BF16)
        c_ones_bf = sb("c_ones_bf", [128, 128], BF16)
        c_neg4_bf = sb("c_neg4_bf", [128, 512], BF16)
        c_prot_bf = sb("c_prot_bf", [128, 128], BF16)
        c_reset = sb("c_reset", [128, TT], BF16)
        c_ident_f = sb("c_ident_f", [128, 128])
        c_ones_f = sb("c_ones_f", [128, 128])
        c_tri_f = sb("c_tri_f", [128, 128])
        c_strict_f = sb("c_strict_f", [128, 128])
        c_causal64 = sb("c_causal64", [64, 64])
        c_dmask = sb("c_dmask", [128, 4, 128])
        c_grow = sb("c_grow", [128, 4, 128])
        c_kscale = sb("c_kscale", [128, 4])
        PV = sb("PV", [128, L, 4, NCH])
        PFIN = sb("PFIN", [128, NCH])
        PLB = sb("PLB", [128, L_FULL, 4])
        LB = sb("LB", [128, L_FULL, 2, 4])
        PHN = sb("PHN", [128, L, 2, 4])
        PCW = sb("PCW", [128, L, NCH, 5])
        PROW = sb("PROW", [128, L, 3, 16])
        PSN = sb("PSN", [128, 1024])
        PFW = sb("PFW", [128, 88, 4])
        DPREV = sb("DPREV", [128, 4])

        PF = stack.enter_context(nc.psum_tensor("PF", [128, 7, 512], F32))
        PB = stack.enter_context(nc.psum_tensor("PB", [128, 1024], BF16))

        IB0, IT0, BC0 = 0, 12 * TT, 28 * TT

        def ibc(i, n=1):
            return BIG[:, IB0 + i * TT:IB0 + (i + n) * TT]

        def IT(a, b, parts=128):
            return BIG[0:parts, IT0 + a:IT0 + b]

        BCv = BIG[:, BC0:BC0 + 8 * TT].rearrange("p (c t) -> p c t", t=TT)
        HID = BIG[:, 0:11 * TT].rearrange("p (c t) -> p c t", t=TT)

        MUL, ADD, SUB = ALU.mult, ALU.add, ALU.subtract

        KCUT = int(os.environ.get("KCUT", "0"))
        marks = []
        build.marks = marks

        def mark(name):
            marks.append((name, len([o for o in P.ops["pe"]])))

        def cut(n):
            if KCUT == n:
                P.mute = True

        def tap(name, ap):
            if name not in taps:
                return
            shp = list(ap.shape)
            t = nc.dram_tensor("tap_" + name, shp, ap.dtype, kind="ExternalOutput").ap()
            tap_out[name] = t
            P.dma(t, ap)

        stage = FT[:, 0, 0:512]
        stage2 = FT[:, 1, 0:512]
        P.dma(c_ident_f[:], cin["ident_f"])
        P.dma(c_ones_f[:], cin["ones_f"])
        P.dma(c_tri_f[:], cin["tri_f"])
        P.dma(c_strict_f[:], cin["strict_f"])
        P.dma(c_causal64[:], cin["causal64"])
        P.dma(c_dmask[:], cin["ret_dmask"])
        P.dma(c_grow[:], cin["ret_grow"])
        P.dma(c_kscale[:], cin["ret_kscale"])
        P.copy(c_ident_bf[:], c_ident_f[:], eng="dve")
        P.copy(c_ones_bf[:], c_ones_f[:], eng="dve")
        P.dma(stage, cin["neg4"])
        P.copy(c_neg4_bf[:], stage, eng="dve")
        P.dma(stage2[:, 0:128], cin["protT"])
        P.copy(c_prot_bf[:], stage2[:, 0:128], eng="dve")
        stage3 = FT[:, 2, 0:512]
        P.dma(stage3, cin["resetmask"])
        P.copy(c_reset[:], stage3, eng="dve")
        P.dma(PV[:], pv)
        P.dma(PFIN[:], pfin)
        P.dma(PLB[:], plb)
        P.dma(PHN[:], phn)
        P.dma(PCW[:], pcw)
        for l in range(L):
            P.dma(PROW[:, l, :, :].rearrange("p a b -> p (a b)"),
                  prow[l].rearrange("a b -> (a b)").partition_broadcast(128))
        for l in range(L):
            P.act(PROW[:, l, 1, :], PROW[:, l, 1, :], AF.Exp)
            P.ts(PROW[:, l, 1, :], PROW[:, l, 1, :], -1.0, MUL)
        EL = SM[:, 0:16].rearrange("p (a b) -> p a b", b=4)
        P.act(EL, PLB[:], AF.Exp)
        P.tt(SM[:, 16:20], EL[:, 0, :], EL[:, 1, :], ADD)
        P.tt(SM[:, 16:20], SM[:, 16:20], EL[:, 2, :], ADD)
        P.tt(SM[:, 16:20], SM[:, 16:20], EL[:, 3, :], ADD)
        P.recip(SM[:, 20:24], SM[:, 16:20])
        for l in range(L_FULL):
            P.tt(EL[:, l, :], EL[:, l, :], SM[:, 20:24], MUL)
        P.memset(LB[:, 0, 0, :], 0.0, eng="dve")
        for l in range(1, L_FULL):
            P.tt(LB[:, l, 0, :], LB[:, l - 1, 0, :], EL[:, l, :], ADD)
        for l in range(L_FULL):
            P.ts(LB[:, l, 1, :], LB[:, l, 0, :], -1.0, MUL, 1.0, ADD)

        def emit_casts(l, pieces, depth):
            P.pool_depth = depth
            for (a, b_) in pieces:
                P.dma(wsb[l][:, a:b_], wsf[l, :, a:b_], eng="pool")

        emit_casts(0, [(a, min(WE, a + 8192)) for a in range(0, WE, 8192)], 12)
        small_pieces = [(a, min(WE, a + 2048)) for a in range(0, WE, 2048)]
        cast_per_tile = -(-len(small_pieces) // NT)

        wstate = {"i": 0, "next": 0}
        wreg = {}
        build.wreg = wreg

        def wload(wname, l, c0, ncols, r0=0, KC=NCH):
            key = (wname, r0, KC, c0, ncols)
            n = KC * ncols
            assert n <= 4096
            if key not in wreg:
                wreg[key] = wstate["next"]
                wstate["next"] += n
            off = wreg[key]
            s = wstate["i"] % NSLOT
            wstate["i"] += 1
            P.dma(WS[:, s, 0:n], wsb[l][:, off:off + n])
            return WS[:, s, 0:n].rearrange("p (k n) -> p k n", n=ncols)

        def fm_slice(wname, l, c0, nchunks, rhs3, KC=NCH, r0=0, cpb=2):
            for b0 in range(0, nchunks, cpb):
                nb_ = min(cpb, nchunks - b0)
                wv = wload(wname, l, c0 + b0 * 128, nb_ * 128, r0=r0, KC=KC)
                for ci in range(nb_):
                    yield b0 + ci, fm_chunk(wv, ci, rhs3, nb(), KC=KC)

        def tm_slice(wname, l, c0, ncols_total, M, ntb, cpb=256):
            for b0 in range(0, ncols_total, cpb):
                wv = wload(wname, l, c0 + b0, cpb)
                for tb in range(ntb):
                    yield tb, b0, tm_block(wv, tb * M, M, nb(), cpb)

        def rmsnorm_fm(src, dst, wcol, ncols, nch=NCH):
            sq = ibc(0)[:, 0:ncols]
            acc = PF[:, 3, 0:ncols]
            for k in range(nch):
                P.act(sq, src[:, k, :], AF.Square)
                P.mm(acc, c_ones_bf[:], sq, start=(k == 0), stop=(k == nch - 1))
            rstd = FT[:, 5, 0:ncols]
            P.act(rstd, acc, AF.Sqrt, bias=EPS, scale=1.0 / (nch * 128))
            P.recip(rstd, rstd)
            if dst is not None:
                for k in range(nch):
                    P.stt(dst[:, k, :], src[:, k, :], wcol[:, k:k + 1], rstd, MUL, MUL)
            return rstd

        def fm_chunk(wv, ci, rhs3, bank, KC=NCH, ncols=TT):
            out = PF[:, bank, 0:ncols]
            for k in range(KC):
                P.mm(out, wv[:, k, ci * 128:(ci + 1) * 128], rhs3[:, k, :], start=(k == 0), stop=(k == KC - 1))
            return out

        def tm_block(wv, tok0, M, bank, ncols, c0=0):
            out = PF[0:M, bank, 0:ncols]
            for k in range(NCH):
                P.mm(out, XN[:, k, tok0:tok0 + M], wv[:, k, c0:c0 + ncols], start=(k == 0), stop=(k == NCH - 1))
            return out

        bank_rr = {"i": 0}

        NB_BANKS = (0, 1, 2, 4, 5, 6)

        def nb():
            b = NB_BANKS[bank_rr["i"] % len(NB_BANKS)]
            bank_rr["i"] += 1
            return b

        def headnorm_out(o_ps, gate, nw, dst, bank=3):
            sq = ibc(0)
            P.act(sq, o_ps, AF.Square)
            ss = PF[:, bank, :]
            P.mm(ss, c_ones_bf[:], sq)
            rstd = FT[:, 5, 0:TT]
            P.act(rstd, ss, AF.Sqrt, bias=EPS, scale=1.0 / 128)
            P.recip(rstd, rstd)
            tmp = FT[:, 4, 0:TT]
            P.tt(tmp, o_ps, rstd, MUL)
            P.stt(dst, tmp, nw, gate, MUL, MUL)

        def wout_partial(l, r0, KC):
            for m, ps in fm_slice("w_out", l, 0, NCH, MIX, KC=KC, r0=r0, cpb=(8 if KC == 4 else 4)):
                P.tt(X[:, m, :], X[:, m, :], ps, ADD)

        OH = [PF[:, 4, :], PF[:, 5, :]]
        GG = BCv[:, 0:4, :]

        for l in range(L):
            last = (l == L - 1)
            src_d = x_in if l == 0 else xs_d
            P.memset(HR[:], 0.0)
            P.memset(RS[:], 0.0)
            P.memset(SS[:], 0.0)
            P.memset(CF[:], 0.0)
            P.memset(CS[:], 0.0)
            P.memset(DPREV[:], 1.0)
            P.dma(PSN[:], psn[l].partition_broadcast(128))
            P.dma(PFW[:], pfw[l])
            if "xattn" in phases:
                memx = X[:, :, 0:NMEM]
                P.dma(memx, mem_in)
                memn = XN[:, :, 0:NMEM]
                rmsnorm_fm(memx, memn, PV[:, l, 2, :], NMEM)
                for hb in range(2):
                    wk_v = wload("xa_wkv", l, hb * 256, 256)
                    for ci in range(2):
                        ps = fm_chunk(wk_v, ci, memn, nb(), ncols=NMEM)
                        P.copy(KT[:, hb * 2 + ci, :], ps)
                for vb in range(2):
                    wv_v = wload("xa_wkv", l, 512 + vb * 256, 256)
                    for mb in range(2):
                        out = PF[:, nb(), 0:256]
                        for k in range(NCH):
                            P.mm(out, memn[:, k, mb * 128:(mb + 1) * 128], wv_v[:, k, :], start=(k == 0), stop=(k == NCH - 1))
                        P.copy(VV[:, mb, vb * 256:(vb + 1) * 256], out)

            for ti in range(NT):
                t0 = ti * TT
                for m in range(NCH):
                    P.dma(X[:, m, :], src_d[ti, :, m, :])
                if l + 1 < L:
                    emit_casts(l + 1, small_pieces[ti * cast_per_tile:(ti + 1) * cast_per_tile], 2)

                if subs:
                    rmsnorm_fm(X[:], XN[:], PV[:, l, 0, :], TT)
                    if l == 0 and ti == 0:
                        tap("xn", XN[:])
                    if "hgrn" in subs:
                        mark("hgrn_proj")
                        QT = ibc(1, 4).rearrange("p (h t) -> p h t", t=TT)
                        KTl = ibc(5, 4).rearrange("p (h t) -> p h t", t=TT)
                        VT = IT(0, 4096, 64).rearrange("p (c n) -> p c n", n=512)
                        for (h, psq), (_, psf) in zip(fm_slice("w_in", l, 0, 4, XN), fm_slice("w_in", l, 512, 4, XN)):
                            qs = FT[:, 0, 0:TT]
                            P.act(qs, psq, AF.Silu)
                            f = FT[:, 1, 0:TT]
                            P.act(f, psf, AF.Sigmoid)
                            P.ts(f, f, LB[:, l, 1, h:h + 1], MUL, LB[:, l, 0, h:h + 1], ADD)
                            g = FT[:, 2, 0:TT]
                            P.act(g, f, AF.Ln)
                            kk = FT[:, 3, 0:TT]
                            P.ts(kk, f, -1.0, MUL, 1.0, ADD)
                            b = FT[:, 1, 0:TT]
                            P.scan(b, c_reset[:], g, 0.0, MUL, ADD)
                            b3 = b.rearrange("p (c t) -> p c t", t=64)
                            bm = SM[:, 32:40]
                            P.copy(bm, b3[:, :, 31], eng="dve")
                            P.tt(b3, b3, bm.unsqueeze(2).to_broadcast([128, 8, 64]), SUB)
                            EM = SM[:, 40:48]
                            Dd = SM[:, 48:56]
                            Ee = SM[:, 64 + h * 8:64 + h * 8 + 8]
                            P.act(EM, bm, AF.Exp)
                            P.act(Dd, b3[:, :, 63], AF.Exp)
                            P.tt(Ee[:, 0:1], EM[:, 0:1], DPREV[:, h:h + 1], MUL)
                            P.tt(Ee[:, 1:8], EM[:, 1:8], Dd[:, 0:7], MUL)
                            P.copy(DPREV[:, h:h + 1], Dd[:, 7:8], eng="dve")
                            e1 = FT[:, 2, 0:TT]
                            P.act(e1, b, AF.Exp)
                            P.tt(QT[:, h, :], qs, e1, MUL)
                            e2 = FT[:, 4, 0:TT]
                            P.act(e2, b, AF.Exp, scale=-1.0)
                            P.tt(KTl[:, h, :], kk, e2, MUL)
                        for c, b0, ps in tm_slice("w_in", l, 1024, 512, 64, 8):
                            P.copy(VT[:, c, b0:b0 + 256], ps)
                        for h, ps in fm_slice("w_in", l, 1536, 4, XN):
                            P.act(GG[:, h, :], ps, AF.Sigmoid)
                        mark("hgrn_batch")
                        KTM = IT(4096, 8192, 64).rearrange("p (h c n) -> p h c n", h=4, c=8)
                        AB = SBF[0:64, 0:2048].rearrange("p (h c n) -> p h c n", h=4, c=8)
                        for h in range(4):
                            pb = PB[0:64, :].rearrange("p (c n) -> p c n", n=128)
                            for c in range(8):
                                P.tr(pb[:, c, :], KTl[:, h, c * 64:(c + 1) * 64], c_ident_bf[:])
                            P.copy(KTM[:, h, :, :], pb)
                            pa = PF[0:64, 6, :].rearrange("p (c n) -> p c n", n=64)
                            for c in range(8):
                                P.mm(pa[:, c, :], KTl[:, h, c * 64:(c + 1) * 64], QT[:, h, c * 64:(c + 1) * 64])
                            P.tt(AB[:, h, :, :], pa, c_causal64[:].unsqueeze(1).to_broadcast([64, 8, 64]), MUL)
                        mark("hgrn_rec")
                        SM8 = ST2[:, :].rearrange("p (b c n) -> p b c n", b=2, c=8)

                        def hg_pp(h):
                            buf = h % 2
                            for c in range(8):
                                P.mm(PF[:, 2 * buf + c // 4, (c % 4) * 128:(c % 4 + 1) * 128],
                                     KTM[:, h, c, :], VT[:, c, h * 128:(h + 1) * 128])
                            for hf in range(2):
                                dst = ST[:, hf * 512:(hf + 1) * 512] if buf == 0 else FT[:, hf, 0:512]
                                P.copy(dst, PF[:, 2 * buf + hf, :])

                        def hg_chain(h):
                            buf = h % 2
                            Ee = SM[:, 64 + h * 8:64 + h * 8 + 8]
                            for c in range(8):
                                pps = ST[:, c * 128:(c + 1) * 128] if buf == 0 else FT[:, c // 4, (c % 4) * 128:(c % 4 + 1) * 128]
                                P.ts(SM8[:, buf, c, :], HR[:, h, :], Ee[:, c:c + 1], MUL)
                                P.stt(HR[:, h, :], HR[:, h, :], Ee[:, c:c + 1], pps, MUL, ADD)

                        def hg_o(h):
                            buf = h % 2
                            for c in range(8):
                                oc = OH[buf][:, c * 64:(c + 1) * 64]
                                P.mm(oc, VT[:, c, h * 128:(h + 1) * 128], AB[:, h, c, :], start=True, stop=False)
                                P.mm(oc, SM8[:, buf, c, :], QT[:, h, c * 64:(c + 1) * 64], start=False, stop=True)

                        def hg_hn(h):
                            headnorm_out(OH[h % 2], GG[:, h, :], PHN[:, l, 0, h:h + 1], MIX[:, h, :], bank=6)

                        hg_pp(0); hg_pp(1); hg_chain(0); hg_chain(1); hg_o(0); hg_o(1)
                        hg_pp(2); hg_pp(3); hg_chain(2); hg_chain(3); hg_hn(0); hg_hn(1)
                        hg_o(2); hg_o(3); hg_hn(2); hg_hn(3)
                        if l == 0 and ti == 0:
                            tap("hg_out", MIX[:, 0:4, :])
                        mark("wout_h")
                        wout_partial(l, 0, 4)

                    if "ret" in subs:
                        mark("ret_proj")
                        ROT = FT[:, 2:4, 0:TT]
                        QR = ibc(1, 4).rearrange("p (h t) -> p h t", t=TT)
                        QH = ibc(5, 4).rearrange("p (h t) -> p h t", t=TT)
                        KR = IT(0, 2048).rearrange("p (h t) -> p h t", t=TT)
                        VR = IT(2048, 4096).rearrange("p (b n) -> p b n", n=512)
                        P.dma(ROT[:, 0, :], cin["rot_cos"][:, t0:t0 + TT])
                        P.dma(ROT[:, 1, :], cin["rot_sin"][:, t0:t0 + TT])
                        for (cq, dst, isq) in ((2048, QR, True), (2560, KR, False)):
                            for h, ps in fm_slice("w_in", l, cq, 4, XN):
                                xb = ibc(9)
                                P.copy(xb, ps)
                                ps2 = PF[:, 3, :]
                                P.mm(ps2, c_prot_bf[:], xb)
                                t1 = FT[:, 0, 0:TT]
                                P.tt(t1, ps, ROT[:, 0, :], MUL)
                                t2 = FT[:, 1, 0:TT]
                                P.tt(t2, ps2, ROT[:, 1, :], MUL)
                                P.tt(dst[:, h, :], t1, t2, ADD, eng="dve")
                                if isq:
                                    P.tt(QH[:, h, :].rearrange("p (c n) -> p c n", n=128),
                                         dst[:, h, :].rearrange("p (c n) -> p c n", n=128),
                                         c_grow[:, h, :].unsqueeze(1).to_broadcast([128, 4, 128]), MUL)
                        cut(1)
                        for bk, b0, ps in tm_slice("w_in", l, 3072, 512, 128, 4):
                            P.copy(VR[:, bk, b0:b0 + 256], ps)
                        for h, ps in fm_slice("w_in", l, 3584, 4, XN):
                            P.act(GG[:, h, :], ps, AF.Silu)
                        cut(2)
                        mark("ret_batch")
                        KHM = IT(4096, 6144).rearrange("p (h c n) -> p h c n", h=4, c=4)
                        AR = IT(6144, 8192).rearrange("p (h c n) -> p h c n", h=4, c=4)
                        for h in range(4):
                            pb = PB[:, 0:512].rearrange("p (c n) -> p c n", n=128)
                            for c in range(4):
                                P.tr(pb[:, c, :], KR[:, h, c * 128:(c + 1) * 128], c_ident_bf[:])
                            P.act(KHM[:, h, :, :], pb, AF.Identity, scale=c_kscale[:, h:h + 1])
                            pa = PF[:, 6, :].rearrange("p (c n) -> p c n", n=128)
                            for c in range(4):
                                P.mm(pa[:, c, :], KR[:, h, c * 128:(c + 1) * 128], QR[:, h, c * 128:(c + 1) * 128])
                            P.tt(AR[:, h, :, :], pa, c_dmask[:, h, :].unsqueeze(1).to_broadcast([128, 4, 128]), MUL)
                        cut(3)
                        mark("ret_rec")
                        SM4 = ST2[:, 0:1024].rearrange("p (b c n) -> p b c n", b=2, c=4)

                        def rt_pp(h):
                            buf = h % 2
                            for c in range(4):
                                P.mm(PF[:, 2 * buf, c * 128:(c + 1) * 128], KHM[:, h, c, :], VR[:, c, h * 128:(h + 1) * 128])
                            dst = ST[:, 0:512] if buf == 0 else FT[:, 0, 0:512]
                            P.copy(dst, PF[:, 2 * buf, :])

                        def rt_chain(h):
                            buf = h % 2
                            for c in range(4):
                                pps = ST[:, c * 128:(c + 1) * 128] if buf == 0 else FT[:, 0, c * 128:(c + 1) * 128]
                                P.copy(SM4[:, buf, c, :], RS[:, h, :], eng="dve")
                                P.stt(RS[:, h, :], RS[:, h, :], hc["ret_g128"][h], pps, MUL, ADD)

                        def rt_o(h):
                            buf = h % 2
                            for c in range(4):
                                oc = OH[buf][:, c * 128:(c + 1) * 128]
                                P.mm(oc, VR[:, c, h * 128:(h + 1) * 128], AR[:, h, c, :], start=True, stop=False)
                                P.mm(oc, SM4[:, buf, c, :], QH[:, h, c * 128:(c + 1) * 128], start=False, stop=True)

                        def rt_hn(h):
                            headnorm_out(OH[h % 2], GG[:, h, :], PHN[:, l, 1, h:h + 1], MIX[:, h, :], bank=6)

                        rt_pp(0); rt_pp(1); rt_chain(0); rt_chain(1); rt_o(0); rt_o(1)
                        rt_pp(2); rt_pp(3); rt_chain(2); rt_chain(3); rt_hn(0); rt_hn(1)
                        rt_o(2); rt_o(3); rt_hn(2); rt_hn(3)
                        P.mute = False
                        if l == 0 and ti == 0:
                            tap("ret_out", MIX[:, 0:4, :])
                        mark("wout_r")
                        wout_partial(l, 4, 4)

                    if "ssd" in subs:
                        mark("ssd_proj")
                        ZS = IT(0, 4096).rearrange("p (b n) -> p b n", n=1024)
                        for bk, b0, ps in tm_slice("w_in", l, 4096, 1024, 128, 4):
                            P.act(ZS[:, bk, b0:b0 + 256], ps, AF.Silu)
                        XBC = ibc(1, 8).rearrange("p (c t) -> p c t", t=TT)
                        BCm = BCv
                        if True:
                            for ch, ps in fm_slice("w_in", l, 5120, 16, XN):
                                U = FT[:, 2 * (ch % 2), 0:TT + 3]
                                P.copy(U[:, 0:3], CS[:, ch, :], eng="dve")
                                P.copy(U[:, 3:TT + 3], ps)
                                acc = FT[:, 2 * (ch % 2) + 1, 0:TT]
                                P.act(acc, ps, AF.Identity, bias=PCW[:, l, ch, 4:5], scale=PCW[:, l, ch, 3:4])
                                for j in range(3):
                                    P.stt(acc, U[:, j:j + TT], PCW[:, l, ch, j:j + 1], acc, MUL, ADD)
                                P.copy(CS[:, ch, :], U[:, TT:TT + 3], eng="dve")
                                dst = XBC[:, ch, :] if ch < 8 else BCm[:, ch - 8, :]
                                P.act(dst, acc, AF.Silu)
                        wv = wload("w_in", l, 7168, 16)
                        pdt = PF[:, 3, 0:64].rearrange("p (b n) -> p b n", n=16)
                        for bk in range(4):
                            for k in range(NCH):
                                P.mm(pdt[:, bk, :], XN[:, k, bk * 128:(bk + 1) * 128], wv[:, k, :], start=(k == 0), stop=(k == NCH - 1))
                        DTS = SM[:, 96:160].rearrange("p (b n) -> p b n", n=16)
                        LA = SM[:, 160:224].rearrange("p (b n) -> p b n", n=16)
                        P.tt(DTS, pdt, PROW[:, l, 0, :].unsqueeze(1).to_broadcast([128, 4, 16]), ADD)
                        P.act(DTS, DTS, AF.Exp)
                        P.act(DTS, DTS, AF.Ln, bias=1.0)
                        P.tt(LA, DTS, PROW[:, l, 1, :].unsqueeze(1).to_broadcast([128, 4, 16]), MUL)
                        mark("ssd_loop")
                        SSB = SBF[:, 0:1024]
                        VHM = SBF[:, 1024:2048]
                        XTM = ST2[:, 0:1024]
                        VTM = ST2[:, 1024:2048]
                        BTM = IT(4096, 4608)
                        GS = IT(4608, 5120).rearrange("p (g n) -> p g n", n=128)
                        LTs = [IT(5120, 5632).rearrange("p (h n) -> p h n", n=128), IT(6144, 6656).rearrange("p (h n) -> p h n", n=128)]
                        MTs = [IT(5632, 6144).rearrange("p (h n) -> p h n", n=128), IT(6656, 7168).rearrange("p (h n) -> p h n", n=128)]
                        YA = [PF[:, 4, :], PF[:, 5, :]]
                        YB = [PF[:, 0, :], PF[:, 1, :]]
                        PSS = [PF[:, 2, :], PF[:, 3, :]]
                        h64 = lambda ap: ap.rearrange("p (h n) -> p h n", n=64)
                        for c in range(4):
                            tk = slice(c * 128, (c + 1) * 128)
                            psm = PF[:, 6, 0:48].rearrange("p (a n) -> p a n", n=16)
                            P.mm(psm[:, 0, :], c_tri_f[:], LA[:, c, :])
                            P.mm(psm[:, 1, :], c_strict_f[:], LA[:, c, :])
                            P.mm(psm[:, 2, :], c_ones_f[:], LA[:, c, :])
                            ECt = SM[:, 224:256].rearrange("p (a n) -> p a n", n=16) if False else FT[:, 5, 0:48].rearrange("p (a n) -> p a n", n=16)
                            P.act(ECt, psm, AF.Exp)
                            pb = PB[:, :].rearrange("p (c n) -> p c n", n=128)
                            for j in range(8):
                                P.tr(pb[:, j, :], XBC[:, j, tk], c_ident_bf[:])
                            P.copy(XTM, PB[:, :])
                            P.tt(h64(VTM), h64(XTM), DTS[:, c, :].unsqueeze(2).to_broadcast([128, 16, 64]), MUL)
                            P.tt(h64(VHM), h64(VTM), ECt[:, 1, :].unsqueeze(2).to_broadcast([128, 16, 64]), MUL)
                            pb2 = PB[:, 0:512].rearrange("p (c n) -> p c n", n=128)
                            for g in range(4):
                                P.tr(pb2[:, g, :], BCm[:, g, tk], c_ident_bf[:])
                            P.copy(BTM, PB[:, 0:512])
                            pg = PF[:, 6, :].rearrange("p (g n) -> p g n", n=128)
                            for g in range(4):
                                P.mm(pg[:, g, :], BCm[:, g, tk], BCm[:, 4 + g, tk])
                            P.copy(GS, pg)
                            def seg_stage(g):
                                segL = ST[:, (g % 2) * 512:(g % 2 + 1) * 512].rearrange("p (h n) -> p h n", n=128)
                                P.tt(segL, c_strict_f[:].unsqueeze(1).to_broadcast([128, 4, 128]),
                                     LA[:, c, 4 * g:4 * g + 4].unsqueeze(2).to_broadcast([128, 4, 128]), MUL)
                                pseg = PF[:, 6 if g % 2 == 0 else 3, :]
                                P.mm(pseg, c_ident_bf[:], c_neg4_bf[:], start=True, stop=False)
                                for hh in range(4):
                                    P.mm(pseg[:, hh * 128:(hh + 1) * 128], segL[:, hh, :], c_tri_f[:], start=False, stop=(hh == 3))

                            def out_stage(g):
                                LT, MT = LTs[g % 2], MTs[g % 2]
                                pseg = PF[:, 6 if g % 2 == 0 else 3, :]
                                P.act(LT, pseg.rearrange("p (h n) -> p h n", n=128), AF.Exp)
                                P.tt(MT, LT, GS[:, g, :].unsqueeze(1).to_broadcast([128, 4, 128]), MUL)
                                for hh in range(4):
                                    hd = 4 * g + hh
                                    P.mm(YA[hd // 8][:, (hd % 8) * 64:(hd % 8) * 64 + 64], MT[:, hh, :], VTM[:, hd * 64:(hd + 1) * 64])

                            seg_stage(0); seg_stage(1); out_stage(0); seg_stage(2); out_stage(1)
                            seg_stage(3); out_stage(2); out_stage(3)
                            P.copy(SSB, SS[:])
                            for g in range(4):
                                P.mm(YB[g // 2][:, (g % 2) * 256:(g % 2) * 256 + 256], BCm[:, 4 + g, tk], SSB[:, g * 256:(g + 1) * 256])
                            for g in range(4):
                                P.mm(PSS[g // 2][:, (g % 2) * 256:(g % 2) * 256 + 256], BTM[:, g * 128:(g + 1) * 128], VHM[:, g * 256:(g + 1) * 256])
                            y = ST[:, :]
                            for hf in range(2):
                                ysl = y[:, hf * 512:(hf + 1) * 512]
                                P.tt(h64(ysl), h64(YB[hf]), ECt[:, 0, hf * 8:hf * 8 + 8].unsqueeze(2).to_broadcast([128, 8, 64]), MUL)
                                P.tt(ysl, YA[hf], ysl, ADD)
                                t3 = FT[:, hf, 0:512]
                                P.tt(h64(t3), h64(XTM[:, hf * 512:(hf + 1) * 512]),
                                     PROW[:, l, 2, hf * 8:hf * 8 + 8].unsqueeze(2).to_broadcast([128, 8, 64]), MUL)
                                P.tt(ysl, ysl, t3, ADD, eng="dve")
                            P.tt(y, y, ZS[:, c, :], MUL)
                            ssq = SM[:, 24:28]
                            junk = FT[:, 2, 0:256]
                            for g in range(4):
                                P.act(junk, y[:, g * 256:(g + 1) * 256], AF.Square, accum_out=ssq[:, g:g + 1])
                            rs = SM[:, 28:32]
                            P.act(rs, ssq, AF.Sqrt, bias=EPS, scale=1.0 / 256)
                            P.recip(rs, rs)
                            y4 = y.rearrange("p (g n) -> p g n", n=256)
                            P.tt(y4, y4, rs.unsqueeze(2).to_broadcast([128, 4, 256]), MUL)
                            YN = ST2[:, 0:1024]
                            P.tt(YN, y, PSN[:], MUL)
                            pbo = PB[:, :].rearrange("p (c n) -> p c n", n=128)
                            for j in range(8):
                                P.tr(pbo[:, j, :], YN[:, j * 128:(j + 1) * 128], c_ident_bf[:])
                            P.copy(MIX[:, 0:8, tk], pbo)
                            P.tt(h64(SS[:]), h64(SS[:]), ECt[:, 2, :].unsqueeze(2).to_broadcast([128, 16, 64]), MUL)
                            for hf in range(2):
                                P.tt(SS[:, hf * 512:(hf + 1) * 512], SS[:, hf * 512:(hf + 1) * 512], PSS[hf], ADD)
                        if l == 0 and ti == 0:
                            tap("ssm_out", MIX[:, 0:8, :])
                        mark("wout_s")
                        wout_partial(l, 8, 8)
                    if l == 0 and ti == 0:
                        tap("x1", X[:])

                if "xattn" in phases:
                    mark("xattn")
                    rmsnorm_fm(X[:], XN[:], PV[:, l, 1, :], TT)
                    QA = ibc(1, 4).rearrange("p (h t) -> p h t", t=TT)
                    AO = ibc(5, 4).rearrange("p (h t) -> p h t", t=TT)
                    for h, ps in fm_slice("xa_wq", l, 0, 4, XN):
                        P.copy(QA[:, h, :], ps)
                    for h in range(4):
                        ET = IT(h * 1024, (h + 1) * 1024).rearrange("p (m t) -> p m t", t=TT)
                        for mb in range(2):
                            pss = PF[:, 4 + mb, :]
                            P.mm(pss, KT[:, h, mb * 128:(mb + 1) * 128], QA[:, h, :])
                            P.act(ET[:, mb, :], pss, AF.Exp, scale=128 ** -0.5)
                        den = PF[:, 6 if h % 2 == 0 else 3, :]
                        P.mm(den, c_ones_bf[:], ET[:, 0, :], start=True, stop=False)
                        P.mm(den, c_ones_bf[:], ET[:, 1, :], start=False, stop=True)
                        rden = FT[:, h, 0:TT]
                        P.recip(rden, den)
                        po = PF[:, nb(), :]
                        P.mm(po, VV[:, 0, h * 128:(h + 1) * 128], ET[:, 0, :], start=True, stop=False)
                        P.mm(po, VV[:, 1, h * 128:(h + 1) * 128], ET[:, 1, :], start=False, stop=True)
                        P.tt(AO[:, h, :], po, rden, MUL)
                    for m, ps in fm_slice("xa_wo", l, 0, NCH, AO, KC=4, cpb=8):
                        P.tt(X[:, m, :], X[:, m, :], ps, ADD)
                    if l == 0 and ti == 0:
                        tap("x2", X[:])

                if "ffn" in phases:
                    mark("ffn")
                    rmsnorm_fm(X[:], XN[:], PV[:, l, 3, :], TT)
                    for (j0, nj) in FFN_PARTS:
                        if True:
                            for (jj, psg), (_, psu) in zip(fm_slice("ffn_w_up", l, j0 * 128, nj, XN),
                                                           fm_slice("ffn_w_up", l, FFN + j0 * 128, nj, XN)):
                                j = j0 + jj
                                res = []
                                for (ps, cidx, slot) in ((psg, j, 0), (psu, 44 + j, 1)):
                                    U = FT[:, slot * 2, 0:TT + 2]
                                    P.copy(U[:, 0:2], CF[:, cidx, :], eng="dve")
                                    P.copy(U[:, 2:TT + 2], ps)
                                    acc = FT[:, slot * 2 + 1, 0:TT]
                                    P.act(acc, ps, AF.Identity, bias=PFW[:, cidx, 3:4], scale=PFW[:, cidx, 2:3])
                                    for q in range(2):
                                        P.stt(acc, U[:, q:q + TT], PFW[:, cidx, q:q + 1], acc, MUL, ADD)
                                    P.copy(CF[:, cidx, :], U[:, TT:TT + 2], eng="dve")
                                    res.append(acc)
                                sg = FT[:, 4, 0:TT]
                                P.act(sg, res[0], AF.Silu)
                                P.tt(HID[:, j - j0, :], sg, res[1], MUL, eng="dve")
                        for m, ps in fm_slice("ffn_w_down", l, 0, NCH, HID, KC=nj, r0=j0):
                            P.tt(X[:, m, :], X[:, m, :], ps, ADD)
                            if (not last) and j0 == FFN_PARTS[-1][0]:
                                P.dma(xs_d[ti, :, m, :], X[:, m, :])
                    if l == 0 and ti == 0:
                        tap("x3", X[:])

                mark("store")
                if last:
                    rstd = rmsnorm_fm(X[:], None, None, TT)
                    for k in range(NCH):
                        P.stt(X[:, k, :], X[:, k, :], PFIN[:, k:k + 1], rstd, MUL, MUL)
                    P.dma(y_out[ti, :, 0:8, :], X[:, 0:8, :])
                    P.dma(y_out[ti, :, 8:16, :], X[:, 8:16, :])
                elif "ffn" not in phases:
                    P.dma(xs_d[ti, :, 0:8, :], X[:, 0:8, :])
                    P.dma(xs_d[ti, :, 8:16, :], X[:, 8:16, :])

        final = P.final_wait_all()
        with nc.Block() as block:
            @block.tensor
            def _(e):
                P.replay("pe", e)

            @block.scalar
            def _(e):
                P.replay("act", e)

            @block.vector
            def _(e):
                P.replay("dve", e)

            @block.gpsimd
            def _(e):
                P.replay("pool", e)

            @block.sync
            def _(e):
                P.replay("sp", e)
                for s, v in final:
                    e.wait_ge(s, v)
        if len(phases) == 3:
            assert wstate["next"] == WE, (wstate["next"], WE)
        build.n_ops = P.n_ops
        build.counts = {e: len(P.ops[e]) for e in P.ENGS}
    return nc


def _fm(v):
    v = np.asarray(v, np.float32)
    lead = v.shape[:-1]
    return np.ascontiguousarray(np.moveaxis(v.reshape(lead + (-1, 128)), -1, 0))


def prep_shared(inp, L, T, wreg):
    hc = host_constants(T)
    sh = {}
    ws = np.zeros((L, 128, WE), np.float32)
    for (wname, r0, KC, c0, ncols), off in wreg.items():
        W = np.asarray(inp[wname], np.float32)
        for l in range(L):
            blk = W[l, r0 * 128:(r0 + KC) * 128, c0:c0 + ncols].reshape(KC, 128, ncols)
            ws[l, :, off:off + KC * ncols] = blk.transpose(1, 0, 2).reshape(128, KC * ncols)
    sh["wstream"] = ws
    pvec = np.stack([_fm(inp["norm_mix"][:L]), _fm(inp["norm_xattn"][:L]), _fm(inp["norm_mem"][:L]),
                     _fm(inp["norm_ffn"][:L])], axis=2)
    sh["pvec"] = np.ascontiguousarray(pvec)
    sh["pfin"] = _fm(inp["norm_final"])
    sh["plb"] = _fm(inp["hg_lb_logits"])
    sh["phn"] = np.ascontiguousarray(np.stack([_fm(inp["hg_norm"][:L]), _fm(inp["ret_norm"][:L])], axis=2))
    cw = _fm(np.asarray(inp["ssm_conv_w"], np.float32)[:L])
    cb = _fm(np.asarray(inp["ssm_conv_b"], np.float32)[:L])
    sh["pcw"] = np.ascontiguousarray(np.concatenate([np.moveaxis(cw, 2, 3), cb[..., None]], axis=3))
    sh["prow"] = np.ascontiguousarray(np.stack([inp["ssm_dt_bias"][:L], inp["ssm_A_log"][:L], inp["ssm_D"][:L]], axis=1).astype(np.float32))
    sh["psn"] = np.ascontiguousarray(np.asarray(inp["ssm_norm"], np.float32)[:L])
    fw = _fm(np.asarray(inp["ffn_conv_w"], np.float32)[:L])
    fb = _fm(np.asarray(inp["ffn_conv_b"], np.float32)[:L])
    sh["pfw"] = np.ascontiguousarray(np.moveaxis(np.concatenate([np.moveaxis(fw, 2, 3), fb[..., None]], axis=3), 1, 0))
    for k in ("ident_f", "ones_f", "tri_f", "strict_f", "neg4", "causal64", "ret_dmask", "ret_grow",
              "ret_kscale", "protT", "rot_cos", "rot_sin", "resetmask"):
        sh["k_" + k] = np.ascontiguousarray(hc[k])
    return sh


def tile_x(xb, T):
    NT = T // TT
    a = np.asarray(xb, np.float32).reshape(NT, TT, NCH, 128)
    return np.ascontiguousarray(a.transpose(0, 3, 2, 1))


def untile_y(yt, T):
    NT = T // TT
    return np.ascontiguousarray(yt.transpose(0, 3, 2, 1)).reshape(T, D)


def kernel(**inp):
    B = inp["x"].shape[0]
    T = inp["x"].shape[1]
    L = inp["w_in"].shape[0]
    nc = build(L, T)
    sh = prep_shared(inp, L, T, build.wreg)
    in_maps = []
    for b in range(B):
        m = dict(sh)
        m["x_t"] = tile_x(inp["x"][b], T)
        m["mem_t"] = _fm(inp["mem"][b]).reshape(128, NMEM, NCH).transpose(0, 2, 1).copy()
        in_maps.append(m)
    res = run_bass_kernel_spmd(nc, in_maps, core_ids=list(range(B)))
    out = np.stack([untile_y(np.asarray(r["y_t"]), T) for r in res.results], axis=0)
    return out.astype(np.float32)
```
